# Optimizing a Trainium2 kernel written in Bass

```python
import math
import jax, jax.numpy as jnp
from jax import lax
import numpy as np

D_MODEL = 1024
BATCH = 32
SEQ = 2048
DEPTH = 1

N_META = 16
HG_HEADS = 4
HG_HEAD_DIM = 128
D_HG = HG_HEADS * HG_HEAD_DIM
HG_CHUNK = 64
D_LRU = D_MODEL
LRU_BLOCKS = 8
LRU_BLOCK = D_LRU // LRU_BLOCKS
CONV_WIDTH = 4
LRU_C = 8.0
N_GROUPS = 4
EXPERTS_PER_GROUP = 8
N_EXPERTS = N_GROUPS * EXPERTS_PER_GROUP
TOP_K = 2
D_EXPERT = 512
MOE_BLOCK = 512
EPS = 1e-6
SPLIT_SIZES = (D_HG, D_HG, D_HG, D_HG, D_LRU, D_LRU, D_MODEL, D_MODEL)
D_IN = sum(SPLIT_SIZES)

kernel_name = "hybrid_hgrn2_rglru_hmoe_block"


def rmsnorm(x, g):
    xf = x.astype(jnp.float32)
    y = xf * lax.rsqrt(jnp.mean(xf * xf, axis=-1, keepdims=True) + EPS)
    return (y * g.astype(jnp.float32)).astype(x.dtype)


def hgrn2_chunked(q, k, v, log_f):
    B, T, H, DK = q.shape
    DV = v.shape[-1]
    C = HG_CHUNK
    NC = T // C

    def to_chunks(a):
        return a.reshape(B, NC, C, H, a.shape[-1]).transpose(1, 0, 3, 2, 4)

    qc, kc, vc, gc = to_chunks(q), to_chunks(k), to_chunks(v), to_chunks(log_f)
    causal = jnp.tril(jnp.ones((C, C), dtype=bool))[:, :, None]

    def step(S, inp):
        qb, kb, vb, gb = inp
        G = jnp.cumsum(gb, axis=2)
        diff = G[:, :, :, None, :] - G[:, :, None, :, :]
        decay = jnp.exp(jnp.where(causal, diff, -jnp.inf))
        scores = jnp.einsum('bhtd,bhsd,bhtsd->bhts', qb, kb, decay)
        o = (jnp.einsum('bhts,bhsv->bhtv', scores, vb)
             + jnp.einsum('bhtd,bhdv->bhtv', qb * jnp.exp(G), S))
        G_end = G[:, :, -1:, :]
        S_new = (jnp.exp(G_end[:, :, 0, :])[..., None] * S
                 + jnp.einsum('bhsd,bhsv->bhdv', kb * jnp.exp(G_end - G), vb))
        return S_new, o

    S0 = jnp.zeros((B, H, DK, DV), jnp.float32)
    _, o = lax.scan(step, S0, (qc, kc, vc, gc))
    return o.transpose(1, 0, 3, 2, 4).reshape(B, T, H, DV)


def hgrn2_branch(q_pre, f_pre, i_pre, og_pre, lb, norm_g):
    B, T, _ = q_pre.shape
    f32 = jnp.float32
    lb = lb.astype(f32)
    fp = f_pre.astype(f32)
    f = lb + (1.0 - lb) * jax.nn.sigmoid(fp)
    log_f = jnp.log(f)
    k = (1.0 - lb) * jax.nn.sigmoid(-fp)
    pad = (-N_META) % HG_CHUNK

    def prep(a):
        a = jnp.pad(a.astype(f32), ((0, 0), (pad, 0), (0, 0)))
        return a.reshape(B, T + pad, HG_HEADS, HG_HEAD_DIM)

    o = hgrn2_chunked(prep(q_pre), prep(k), prep(i_pre), prep(log_f))[:, pad:]
    o = o * lax.rsqrt(jnp.mean(o * o, axis=-1, keepdims=True) + EPS)
    o = o * norm_g.astype(f32).reshape(HG_HEADS, HG_HEAD_DIM)
    o = o.reshape(B, T, D_HG) * jax.nn.sigmoid(og_pre.astype(f32))
    return o.astype(q_pre.dtype)


def rglru_branch(xb, yb, conv_w, conv_b, w_r, b_r, w_i, b_i, lam):
    B, T, _ = xb.shape
    f32 = jnp.float32
    xc = lax.conv_general_dilated(
        xb, conv_w[:, None, :].astype(xb.dtype), window_strides=(1,),
        padding=[(CONV_WIDTH - 1, 0)], dimension_numbers=('NWC', 'WIO', 'NWC'),
        feature_group_count=D_LRU) + conv_b.astype(xb.dtype)
    xh = xc.reshape(B, T, LRU_BLOCKS, LRU_BLOCK)
    r = jax.nn.sigmoid(jnp.einsum('btni,nij->btnj', xh, w_r).reshape(B, T, D_LRU).astype(f32)
                       + b_r.astype(f32))
    i = jax.nn.sigmoid(jnp.einsum('btni,nij->btnj', xh, w_i).reshape(B, T, D_LRU).astype(f32)
                       + b_i.astype(f32))
    log_a = -LRU_C * r * jax.nn.softplus(-lam.astype(f32))
    a = jnp.exp(log_a)
    u = jnp.sqrt(-jnp.expm1(2.0 * log_a)) * (i * xc.astype(f32))

    def combine(left, right):
        a_l, b_l = left
        a_r, b_r2 = right
        return a_l * a_r, a_r * b_l + b_r2

    _, h = lax.associative_scan(combine, (a, u), axis=1)
    return (h * jax.nn.gelu(yb.astype(f32))).astype(xb.dtype)


def hier_moe(h, w_group, b_group, w_router, b_router, w_gate, w_up, w_down):
    B, T, D = h.shape
    N = B * T
    f32 = jnp.float32
    hf = h.reshape(N, D)
    group_logits = (hf @ w_group).astype(f32) + b_group.astype(f32)
    p_group = jax.nn.softmax(group_logits, axis=-1)
    g_sel = jnp.argmax(group_logits, axis=-1).astype(jnp.int32)
    p_sel = jnp.take_along_axis(p_group, g_sel[:, None], axis=-1)
    exp_logits = jnp.einsum('nd,gde->nge', hf, w_router).astype(f32) + b_router.astype(f32)
    sel_logits = jnp.take_along_axis(exp_logits, g_sel[:, None, None], axis=1)[:, 0]
    top_vals, top_idx = lax.top_k(sel_logits, TOP_K)
    weights = p_sel * jax.nn.softmax(top_vals, axis=-1)
    expert_ids = g_sel[:, None] * EXPERTS_PER_GROUP + top_idx.astype(jnp.int32)

    A = N * TOP_K
    e_flat = expert_ids.reshape(A)
    w_flat = weights.reshape(A)
    tok_flat = jnp.arange(A, dtype=jnp.int32) // TOP_K
    order = jnp.argsort(e_flat)
    e_sorted = e_flat[order]
    counts = jnp.zeros((N_EXPERTS,), jnp.int32).at[e_flat].add(1)
    starts = jnp.cumsum(counts) - counts
    pcounts = (counts + MOE_BLOCK - 1) // MOE_BLOCK * MOE_BLOCK
    pends = jnp.cumsum(pcounts)
    pstarts = pends - pcounts
    dest = pstarts[e_sorted] + (jnp.arange(A, dtype=jnp.int32) - starts[e_sorted])
    n_blocks = -(-A // MOE_BLOCK) + N_EXPERTS
    n_slots = n_blocks * MOE_BLOCK
    slot_tok = jnp.zeros((n_slots,), jnp.int32).at[dest].set(tok_flat[order])
    slot_w = jnp.zeros((n_slots,), f32).at[dest].set(w_flat[order])
    block_start = jnp.arange(n_blocks, dtype=jnp.int32) * MOE_BLOCK
    block_expert = jnp.minimum(jnp.searchsorted(pends, block_start, side='right'),
                               N_EXPERTS - 1).astype(jnp.int32)

    def run_block(args):
        toks, wts, e = args
        xb = hf[toks]
        y = (jax.nn.silu(xb @ w_gate[e]) * (xb @ w_up[e])) @ w_down[e]
        return y * wts[:, None].astype(y.dtype)

    ys = lax.map(run_block, (slot_tok.reshape(n_blocks, MOE_BLOCK),
                             slot_w.reshape(n_blocks, MOE_BLOCK), block_expert))
    out = jnp.zeros((N, D), h.dtype).at[slot_tok].add(ys.reshape(n_slots, D).astype(h.dtype))
    return out.reshape(B, T, D)


def mixer(hn, w_in, lb, hg_norm_g, conv_w, conv_b, w_r, b_r, w_i, b_i, lam, w_up_a, w_up_b, w_out):
    proj = hn @ w_in
    idx = [int(c) for c in np.cumsum(SPLIT_SIZES)[:-1]]
    q_pre, f_pre, i_pre, og_pre, lx, ly, ga, gb = jnp.split(proj, idx, axis=-1)
    o_a = hgrn2_branch(q_pre, f_pre, i_pre, og_pre, lb, hg_norm_g)
    o_b = rglru_branch(lx, ly, conv_w, conv_b, w_r, b_r, w_i, b_i, lam)
    merged = jax.nn.sigmoid(ga) * (o_a @ w_up_a) + jax.nn.sigmoid(gb) * (o_b @ w_up_b)
    return merged @ w_out


def setup_inputs(seed: int = 0) -> dict:
    key = jax.random.key(seed)
    ks = jax.random.split(key, 26)
    f32 = jnp.float32
    nrm = lambda k, shape, scale: jax.random.normal(k, shape, f32) * scale
    u = jax.random.uniform(ks[13], (DEPTH, D_LRU), f32, minval=0.9, maxval=0.999)
    a0 = u ** (1.0 / LRU_C)
    return {
        "x": nrm(ks[0], (BATCH, SEQ, D_MODEL), 1.0),
        "meta_tokens": nrm(ks[1], (N_META, D_MODEL), 1.0),
        "norm1_g": 1.0 + nrm(ks[2], (DEPTH, D_MODEL), 0.02),
        "w_in": nrm(ks[3], (DEPTH, D_MODEL, D_IN), D_MODEL ** -0.5),
        "hg_lower_bounds": nrm(ks[4], (DEPTH + 1, D_HG), 0.1),
        "hg_norm_g": 1.0 + nrm(ks[5], (DEPTH, D_HG), 0.02),
        "conv_w": nrm(ks[6], (DEPTH, CONV_WIDTH, D_LRU), CONV_WIDTH ** -0.5),
        "conv_b": nrm(ks[7], (DEPTH, D_LRU), 0.01),
        "lru_w_r": nrm(ks[8], (DEPTH, LRU_BLOCKS, LRU_BLOCK, LRU_BLOCK), LRU_BLOCK ** -0.5),
        "lru_b_r": nrm(ks[9], (DEPTH, D_LRU), 0.01),
        "lru_w_i": nrm(ks[10], (DEPTH, LRU_BLOCKS, LRU_BLOCK, LRU_BLOCK), LRU_BLOCK ** -0.5),
        "lru_b_i": nrm(ks[11], (DEPTH, D_LRU), 0.01),
        "lru_lambda": jnp.log(a0) - jnp.log1p(-a0),
        "w_up_a": nrm(ks[14], (DEPTH, D_HG, D_MODEL), D_HG ** -0.5),
        "w_up_b": nrm(ks[15], (DEPTH, D_LRU, D_MODEL), D_LRU ** -0.5),
        "w_out": nrm(ks[16], (DEPTH, D_MODEL, D_MODEL), D_MODEL ** -0.5),
        "norm2_g": 1.0 + nrm(ks[17], (DEPTH, D_MODEL), 0.02),
        "w_group": nrm(ks[18], (DEPTH, D_MODEL, N_GROUPS), D_MODEL ** -0.5),
        "b_group": nrm(ks[19], (DEPTH, N_GROUPS), 0.01),
        "w_router": nrm(ks[20], (DEPTH, N_GROUPS, D_MODEL, EXPERTS_PER_GROUP), D_MODEL ** -0.5),
        "b_router": nrm(ks[21], (DEPTH, N_GROUPS, EXPERTS_PER_GROUP), 0.01),
        "w_gate": nrm(ks[22], (DEPTH, N_EXPERTS, D_MODEL, D_EXPERT), D_MODEL ** -0.5),
        "w_up": nrm(ks[23], (DEPTH, N_EXPERTS, D_MODEL, D_EXPERT), D_MODEL ** -0.5),
        "w_down": nrm(ks[24], (DEPTH, N_EXPERTS, D_EXPERT, D_MODEL), D_EXPERT ** -0.5),
        "final_g": 1.0 + nrm(ks[25], (D_MODEL,), 0.02),
    }


def reference(x, meta_tokens, norm1_g, w_in, hg_lower_bounds, hg_norm_g, conv_w, conv_b,
              lru_w_r, lru_b_r, lru_w_i, lru_b_i, lru_lambda, w_up_a, w_up_b, w_out,
              norm2_g, w_group, b_group, w_router, b_router, w_gate, w_up, w_down, final_g):
    B = x.shape[0]
    meta = jnp.broadcast_to(meta_tokens[None].astype(x.dtype), (B, N_META, D_MODEL))
    h = jnp.concatenate([meta, x], axis=1)
    lbs = jnp.cumsum(jax.nn.softmax(hg_lower_bounds.astype(jnp.float32), axis=0), axis=0)
    for l in range(DEPTH):
        mix = mixer(rmsnorm(h, norm1_g[l]), w_in[l], lbs[l], hg_norm_g[l], conv_w[l], conv_b[l],
                    lru_w_r[l], lru_b_r[l], lru_w_i[l], lru_b_i[l], lru_lambda[l],
                    w_up_a[l], w_up_b[l], w_out[l])
        h = h + mix.astype(h.dtype)
        ffn = hier_moe(rmsnorm(h, norm2_g[l]), w_group[l], b_group[l], w_router[l], b_router[l],
                       w_gate[l], w_up[l], w_down[l])
        h = h + ffn.astype(h.dtype)
    return rmsnorm(h, final_g)[:, N_META:]
```

```python
from contextlib import ExitStack
import numpy as np
import concourse.bass as bass
import concourse.mybir as mybir
from concourse.bass_utils import run_bass_kernel_spmd

F32 = mybir.dt.float32
BF16 = mybir.dt.bfloat16
I32 = mybir.dt.int32
AF = mybir.ActivationFunctionType
ALU = mybir.AluOpType
AX = mybir.AxisListType

D = 1024
SEQ = 2048
NMETA = 16
DIN = 6144
NE = 32
DE = 512
EPS = 1e-6
NCORES = 8
BLK = 512

ENGS = ("sp", "act", "dve", "pool", "pe")
SEM_ROLL = 30000
DMA_RING = 16


ACT_SETS = {
    AF.Exp: (0, 6, 22), AF.Tanh: (0, 2, 8, 10, 11, 12, 18, 19, 20), AF.Sigmoid: (2, 21), AF.Sqrt: (3, 23),
    AF.Ln: (5, 6), AF.Gelu_apprx_tanh: (11,), AF.Silu: (18,),
}


class Buf:
    __slots__ = ("name", "lw", "rd", "excl")

    def __init__(self, name=""):
        self.name = name
        self.lw = None
        self.rd = []
        self.excl = False


class Tl:
    __slots__ = ("ap", "b")

    def __init__(self, ap, name=""):
        self.ap = ap
        self.b = Buf(name)


class Op:
    __slots__ = ("eng", "fn", "deps", "dma", "has_dep", "sem", "val", "prewait", "alld", "cost", "lat", "seg", "gi", "tbl", "preload")

    def __init__(self, eng, fn, dma):
        self.eng = eng
        self.fn = fn
        self.dma = dma
        self.deps = []
        self.alld = []
        self.has_dep = False
        self.sem = None
        self.val = 0
        self.prewait = None
        self.cost = 100.0
        self.lat = 0.0
        self.seg = 0
        self.gi = 0
        self.tbl = None
        self.preload = False


def _b(x):
    return x.b if isinstance(x, Tl) else x


class Sched:
    def __init__(self, nc):
        self.nc = nc
        self.ops = {e: [] for e in ENGS}
        self.allbufs = []
        self.seg = 0
        self.gi = 0
        self.reorder = True

    def op(self, eng, fn, reads=(), writes=(), dma=False, cost=None, lat=None):
        reads = [_b(x) for x in reads]
        writes = [_b(x) for x in writes]
        writes = writes + [b for b in reads if b.excl]
        reads = [b for b in reads if not b.excl]
        o = Op(eng, fn, dma)
        deps = {}
        for b in reads:
            if b.lw is not None:
                deps[id(b.lw)] = b.lw
        for b in writes:
            if b.lw is not None:
                deps[id(b.lw)] = b.lw
            for r in b.rd:
                deps[id(r)] = r
        out = []
        for d in deps.values():
            if (not d.dma) and d.eng == eng and not dma and eng == "pe":
                continue
            out.append(d)
        o.deps = out
        o.alld = list(deps.values())
        o.seg = self.seg
        o.gi = self.gi
        self.gi += 1
        if cost is not None:
            o.cost = cost
        if lat is not None:
            o.lat = lat
        for b in reads:
            b.rd.append(o)
        for b in writes:
            b.lw = o
            b.rd = []
        self.ops[eng].append(o)
        return o

    def list_schedule(self):
        import heapq
        allops = sorted((o for e in ENGS for o in self.ops[e]), key=lambda o: o.gi)
        segs = {}
        for o in allops:
            segs.setdefault(o.seg, []).append(o)
        new = {e: [] for e in ENGS}
        SEMLAT = 250.0
        cur_tbl = [-1]
        for sg in sorted(segs):
            ops = segs[sg]
            if len(ops) < 40:
                for o in ops:
                    new[o.eng].append(o)
                continue
            idx = {id(o): i for i, o in enumerate(ops)}
            n = len(ops)
            preds = [[idx[id(d)] for d in o.alld if id(d) in idx] for o in ops]
            succ = [[] for _ in range(n)]
            for i, ps in enumerate(preds):
                for p in ps:
                    succ[p].append(i)
            prio = [0.0] * n
            for i in range(n - 1, -1, -1):
                o = ops[i]
                m = 0.0
                for s_ in succ[i]:
                    if prio[s_] > m:
                        m = prio[s_]
                prio[i] = m + o.cost + o.lat
            indeg = [len(p) for p in preds]
            ready_t = [0.0] * n
            fin = [0.0] * n
            free = {e: 0.0 for e in ENGS}
            wait_h = {e: [] for e in ENGS}
            rdy_h = {e: [] for e in ENGS}
            for i in range(n):
                if indeg[i] == 0:
                    heapq.heappush(wait_h[ops[i].eng], (0.0, i))
            done = 0
            while done < n:
                best = None
                for e in ENGS:
                    t = free[e]
                    if rdy_h[e]:
                        cand = t
                    elif wait_h[e]:
                        cand = max(t, wait_h[e][0][0])
                    else:
                        continue
                    if best is None or cand < best[0]:
                        best = (cand, e)
                t, e = best
                while wait_h[e] and wait_h[e][0][0] <= t:
                    rt, i = heapq.heappop(wait_h[e])
                    heapq.heappush(rdy_h[e], (-prio[i], i))
                sw = 0.0
                if e == "act" and len(rdy_h[e]) > 1:
                    pick = None
                    for ent in rdy_h[e]:
                        tb = ops[ent[1]].tbl
                        if tb is None or cur_tbl[0] in tb:
                            if pick is None or ent < pick:
                                pick = ent
                    if pick is None:
                        pick = min(rdy_h[e])
                    rdy_h[e].remove(pick)
                    heapq.heapify(rdy_h[e])
                    i = pick[1]
                else:
                    _, i = heapq.heappop(rdy_h[e])
                if e == "act" and ops[i].tbl is not None and cur_tbl[0] not in ops[i].tbl and wait_h[e]:
                    alt = None
                    for ent in wait_h[e]:
                        if ent[0] <= t + 1300.0:
                            tb = ops[ent[1]].tbl
                            if tb is None or cur_tbl[0] in tb:
                                if alt is None or ent < alt:
                                    alt = ent
                    if alt is not None:
                        heapq.heappush(rdy_h[e], (-prio[i], i))
                        wait_h[e].remove(alt)
                        heapq.heapify(wait_h[e])
                        i = alt[1]
                o = ops[i]
                if e == "act" and o.tbl is not None and cur_tbl[0] not in o.tbl:
                    cur_tbl[0] = o.tbl[0]
                    sw = 1300.0
                start = max(t, ready_t[i]) + sw
                free[e] = start + o.cost
                fin[i] = start + o.cost + o.lat
                new[e].append(o)
                done += 1
                for s_ in succ[i]:
                    so = ops[s_]
                    r = fin[i] + (SEMLAT if (so.eng != e or o.dma) else 60.0)
                    if so.eng == e and e == "pe" and not o.dma:
                        r = start + o.cost
                    if r > ready_t[s_]:
                        ready_t[s_] = r
                    indeg[s_] -= 1
                    if indeg[s_] == 0:
                        heapq.heappush(wait_h[so.eng], (ready_t[s_], s_))
        self.ops = new

    def emit(self, stack):
        nc = self.nc
        if self.reorder:
            self.list_schedule()
        pos = {}
        for e in ENGS:
            for i, o in enumerate(self.ops[e]):
                pos[id(o)] = i
        for e in ENGS:
            for o in self.ops[e]:
                best = {}
                need = []
                for d in o.deps:
                    if d.dma:
                        need.append(d)
                    else:
                        c = best.get(d.eng)
                        if c is None or pos[id(d)] > pos[id(c)]:
                            best[d.eng] = d
                need.extend(best.values())
                o.deps = need
                for d in need:
                    d.has_dep = True
        esems = {}
        for e in ENGS:
            nmile = sum(1 for o in self.ops[e] if (not o.dma) and o.has_dep)
            ngen = nmile // SEM_ROLL + 1
            esems[e] = [stack.enter_context(nc.semaphore(f"s_{e}_{g}")) for g in range(ngen)]
        rsems = {}
        for e in ENGS:
            if any(o.dma for o in self.ops[e]):
                rsems[e] = [stack.enter_context(nc.semaphore(f"r_{e}_{j}")) for j in range(DMA_RING)]
        finals = []
        for e in ENGS:
            cnt = 0
            dcnt = 0
            for o in self.ops[e]:
                if o.dma:
                    j = dcnt % DMA_RING
                    o.sem = rsems[e][j]
                    o.val = 16 * (dcnt // DMA_RING + 1)
                    if dcnt >= DMA_RING:
                        o.prewait = (o.sem, o.val - 16)
                    dcnt += 1
                elif o.has_dep:
                    o.sem = esems[e][cnt // SEM_ROLL]
                    o.val = cnt % SEM_ROLL + 1
                    cnt += 1
            if e in rsems:
                for j in range(min(dcnt, DMA_RING)):
                    uses = (dcnt - j + DMA_RING - 1) // DMA_RING
                    finals.append((rsems[e][j], 16 * uses))
        block = stack.enter_context(nc.Block())

        def run(e, name):
            seen = {}
            for o in self.ops[name]:
                waits = []
                if o.prewait is not None:
                    waits.append(o.prewait)
                for d in o.deps:
                    waits.append((d.sem, d.val))
                for s, v in waits:
                    k = id(s)
                    if seen.get(k, 0) >= v:
                        continue
                    seen[k] = v
                    e.wait_ge(s, v)
                ins = o.fn(e)
                if o.dma:
                    ins.then_inc(o.sem, 16)
                elif o.has_dep:
                    ins.then_inc(o.sem, 1)
            if name == "sp":
                for s, v in finals:
                    e.wait_ge(s, v)

        @block.sync
        def _(e):
            run(e, "sp")

        @block.scalar
        def _(e):
            run(e, "act")

        @block.vector
        def _(e):
            run(e, "dve")

        @block.gpsimd
        def _(e):
            run(e, "pool")

        @block.tensor
        def _(e):
            run(e, "pe")


class Arena:
    def __init__(self, ap):
        self.ap = ap
        self.off = 0
        self.W = ap.shape[1]

    def alloc(self, free, dt=F32, parts=128, name=""):
        if isinstance(free, int):
            free = (free,)
        n = 1
        for f in free:
            n *= f
        two = dt == BF16
        words = (n + 1) // 2 if two else n
        assert self.off + words <= self.W, f"arena overflow {name} {self.off}+{words}>{self.W}"
        a = self.ap[0:parts, self.off:self.off + words]
        self.off += words
        if two:
            a = a.bitcast(BF16)
            if n != 2 * words:
                a = a[:, 0:n]
        elif dt != F32:
            a = a.bitcast(dt)
        if len(free) == 2:
            a = a.rearrange("p (a b) -> p a b", a=free[0])
        elif len(free) == 3:
            a = a.rearrange("p (a b c) -> p a b c", a=free[0], b=free[1])
        return Tl(a, name)


class KB:
    def __init__(self, nseq, dbg=(), stop=None):
        self.nseq = nseq
        self.ntok = nseq * SEQ
        self.dbg = set(dbg)
        self.stop = stop
        self.nc = bass.Bass("TRN2", target_bir_lowering=False)
        self.S = Sched(self.nc)

    @staticmethod
    def _n(ap):
        n = 1
        for d in ap.shape[1:]:
            n *= d
        return n

    def _ew_cost(self, eng, out, in_=None, mult=1.0):
        n = self._n(out)
        if eng == "act":
            return 190.0 + 0.72 * n * mult
        if eng == "dve":
            return 75.0 + 1.04 * n * mult
        return 130.0 + 1.2 * n * mult

    def act(self, out, in_, func, R, W, **kw):
        o = self.S.op("act", lambda e: e.activation(out=out, in_=in_, func=func, **kw), R, W, cost=self._ew_cost("act", out), lat=60.0)
        o.tbl = ACT_SETS.get(func)
        return o

    def ts(self, eng, out, in0, s1, s2, op0, op1, R, W):
        c = self._ew_cost(eng, out, mult=0.6 if eng == "dve" else 1.0)
        if s2 is None:
            return self.S.op(eng, lambda e: e.tensor_scalar(out=out, in0=in0, scalar1=s1, scalar2=None, op0=op0), R, W, cost=c, lat=60.0)
        return self.S.op(eng, lambda e: e.tensor_scalar(out=out, in0=in0, scalar1=s1, scalar2=s2, op0=op0, op1=op1), R, W, cost=c, lat=60.0)

    def tt(self, eng, out, in0, in1, op, R, W):
        return self.S.op(eng, lambda e: e.tensor_tensor(out=out, in0=in0, in1=in1, op=op), R, W, cost=self._ew_cost(eng, out), lat=60.0)

    def stt(self, out, in0, scalar, in1, op0, op1, R, W):
        return self.S.op("dve", lambda e: e.scalar_tensor_tensor(out=out, in0=in0, scalar=scalar, in1=in1, op0=op0, op1=op1), R, W, cost=self._ew_cost("dve", out), lat=60.0)

    def cp(self, eng, out, in_, R, W):
        c = self._ew_cost(eng, out, mult=0.7 if eng == "dve" else 1.0)
        if eng == "act":
            return self.S.op("act", lambda e: e.copy(out=out, in_=in_), R, W, cost=c, lat=60.0)
        return self.S.op(eng, lambda e: e.tensor_copy(out=out, in_=in_), R, W, cost=c, lat=60.0)

    def mm(self, out, lhsT, rhs, start, stop, R, W):
        n = self._n(out)
        c = (max(n, 64) * 0.43 + 12.0) * (4.0 if lhsT.dtype == F32 else 1.0)
        return self.S.op("pe", lambda e: e.matmul(out, lhsT=lhsT, rhs=rhs, start=start, stop=stop), R, W, cost=c, lat=220.0)

    def tr(self, out, in_, ident, R, W):
        c = 70.0 * (4.0 if in_.dtype == F32 else 1.0)
        return self.S.op("pe", lambda e: e.transpose(out, in_, ident), R, W, cost=c, lat=220.0)

    def scan(self, out, d0, d1, init, R, W):
        return self.S.op("dve", lambda e: e.tensor_tensor_scan(out=out, data0=d0, data1=d1, initial=init, op0=ALU.mult, op1=ALU.add), R, W,
                         cost=self._ew_cost("dve", out, mult=2.0), lat=60.0)

    def recip(self, out, in_, R, W):
        return self.S.op("dve", lambda e: e.reciprocal(out=out, in_=in_), R, W, cost=self._ew_cost("dve", out, mult=6.3), lat=60.0)

    def memset(self, eng, ap, val, W):
        return self.S.op(eng, lambda e: e.memset(ap, val), [], W, cost=self._ew_cost(eng, ap, mult=0.5))

    def dma(self, eng, out, in_, R, W, **kw):
        nbytes = self._n(out) * out.shape[0] * (2 if out.dtype == BF16 else 4)
        return self.S.op(eng, lambda e: e.dma_start(out=out, in_=in_, **kw), R, W, dma=True, cost=70.0, lat=2200.0 + nbytes / 120.0)

    def idma(self, fn, nbytes, R, W):
        return self.S.op("pool", fn, R, W, dma=True, cost=1100.0, lat=3000.0 + nbytes / 90.0)

    def dma_out(self, eng, out, in_, R, **kw):
        t = self.track(Tl(None, "dram_w"))
        return self.dma(eng, out, in_, R, [t], **kw)

    def barrier(self, tiles):
        S = self.S
        S.seg += 1
        allb = list(self._all_bufs)
        tok = self.bar_tok
        self.memset("dve", tok.ap[:, 0:4], 0.0, allb + [tok])
        self.memset("pool", tok.ap[:, 1:2], 0.0, [tok] + allb)
        self.S.op("act", lambda e: e.copy(out=tok.ap[:, 2:3], in_=tok.ap[:, 0:1]), [tok], [tok] + allb)
        self.S.op("pe", lambda e: e.matmul(self.pb[6].ap[0:1, 0:1], lhsT=self.bar_bf.ap[0:1, 0:1], rhs=self.bar_bf.ap[0:1, 0:1], start=True, stop=True), [tok, self.bar_bf], [self.pb[6]] + allb)
        self.dma("sp", self.bar_d[0:1, 0:4], tok.ap[0:1, 0:4], [tok] + allb, [tok])
        self.S.op("dve", lambda e: e.memset(tok.ap[:, 3:4], 0.0), [tok], [tok] + allb)
        S.seg += 1

    def track(self, t):
        self._all_bufs.append(t.b)
        return t

    def build(self):
        nc = self.nc
        S = self.S
        NT = self.ntok

        def din(name, shape, dt=F32):
            return nc.dram_tensor(name, list(shape), dt, kind="ExternalInput").ap()

        def dscr(name, shape, dt):
            kind = "ExternalOutput" if name in self.dbg else "Internal"
            return nc.dram_tensor(name, list(shape), dt, kind=kind).ap()

        I = {}
        I["x"] = din("x", [NT, D])
        I["meta_tokens"] = din("meta_tokens", [NMETA, D])
        I["norm1_g"] = din("norm1_g", [D])
        I["w_in"] = din("w_in", [D, DIN])
        I["hg_lower_bounds"] = din("hg_lower_bounds", [2, 512])
        I["hg_norm_g"] = din("hg_norm_g", [512])
        I["conv_w"] = din("conv_w", [4, D])
        I["conv_b"] = din("conv_b", [D])
        I["lru_w_r"] = din("lru_w_r", [8, 128, 128])
        I["lru_b_r"] = din("lru_b_r", [D])
        I["lru_w_i"] = din("lru_w_i", [8, 128, 128])
        I["lru_b_i"] = din("lru_b_i", [D])
        I["lru_lambda"] = din("lru_lambda", [D])
        I["w_up_a"] = din("w_up_a", [512, D])
        I["w_up_b"] = din("w_up_b", [D, D])
        I["w_out"] = din("w_out", [D, D])
        I["norm2_g"] = din("norm2_g", [D])
        I["w_group"] = din("w_group", [D, 4])
        I["b_group"] = din("b_group", [4])
        I["w_router"] = din("w_router", [4, D, 8])
        I["b_router"] = din("b_router", [32])
        I["w_gate"] = din("w_gate", [NE, D, DE])
        I["w_up"] = din("w_up", [NE, D, DE])
        I["w_down"] = din("w_down", [NE, DE, D])
        I["final_g"] = din("final_g", [D])
        self.I = I
        out_kind = "ExternalOutput"
        self.out_d = nc.dram_tensor("out", [NT, D], F32, kind=out_kind).ap()
        self.hnT_d = dscr("hnT_d", [D, NT], BF16)
        self.hnTm_d = dscr("hnTm_d", [D, NMETA], BF16)
        self.ta_d = dscr("ta_d", [D, NT], BF16)
        self.h2_d = dscr("h2_d", [NT, D], F32)
        self.hn2_d = dscr("hn2_d", [NT, D], BF16)
        self.bar_d = dscr("bar_d", [1, 4], F32)
        NBk = (2 * NT) // BLK + NE
        self.slot_d = dscr("slot_d", [NBk * BLK, 4], I32)
        self.Y_d = dscr("Y_d", [NBk * BLK, D], F32)
        self.DB = {}
        for nm in self.dbg:
            pass

        with ExitStack() as st:
            big = st.enter_context(nc.sbuf_tensor("arena", [128, 52000], F32))
            self.big = big
            self.pb = []
            for i in range(8):
                p = st.enter_context(nc.psum_tensor(f"pb{i}", [128, 512], F32))
                self.pb.append(Tl(p[:], f"pb{i}"))
                self.pb[-1].b.excl = True
            self._all_bufs = [p.b for p in self.pb]
            self.ar = Arena(big[:])
            ar = self.ar
            self.bar_tok = ar.alloc(4, F32, name="bar_tok")
            self.bar_bf = self.track(ar.alloc(2, BF16, name="bar_bf"))
            self.memset("dve", self.bar_bf.ap, 0.0, [self.bar_bf])
            self.consts()
            NTL = self.ntok // 128
            self.Mall = self.track(ar.alloc((NTL, 64), BF16, name="Mall"))
            self.wts = self.track(ar.alloc((NTL, 2), F32, name="wts"))
            self.NB = (2 * self.ntok) // BLK + NE
            self.bei = self.track(ar.alloc((self.NB, 2), I32, name="bei"))
            self.dsti = self.track(ar.alloc((NTL, 2), I32, name="dsti"))
            base = ar.off
            self.phase_a1()
            if self.stop and self.stop.startswith("a1"):
                S.emit(st)
                return nc
            self.barrier(None)
            ar.off = base
            self.phase_a2()
            if self.stop and self.stop.startswith("a2"):
                S.emit(st)
                return nc
            self.barrier(None)
            ar.off = base
            self.phase_b0()
            if self.stop == "b0":
                S.emit(st)
                return nc
            self.barrier(None)
            ar.off = base
            self.phase_b()
            if self.stop == "b":
                S.emit(st)
                return nc
            self.barrier(None)
            ar.off = base
            self.phase_c()
            S.emit(st)
        return nc

    def consts(self):
        ar = self.ar
        T = self.track
        self.ident_bf = T(ar.alloc(128, BF16, name="ident_bf"))
        self.ident_f = T(ar.alloc(128, F32, name="ident_f"))
        self.ones_f = T(ar.alloc(128, F32, name="ones_f"))
        self.eps_t = T(ar.alloc(1, F32, name="eps"))
        self.one_t = T(ar.alloc(1, F32, name="one"))
        self.memset("pool", self.ident_f.ap, 1.0, [self.ident_f])
        self.S.op("pool", lambda e: e.affine_select(out=self.ident_f.ap, in_=self.ident_f.ap, pattern=[[1, 128]], compare_op=ALU.is_equal, fill=0.0, base=0, channel_multiplier=-1), [self.ident_f], [self.ident_f])
        self.cp("dve", self.ident_bf.ap, self.ident_f.ap, [self.ident_f], [self.ident_bf])
        self.memset("dve", self.ones_f.ap, 1.0, [self.ones_f])
        self.memset("dve", self.eps_t.ap, EPS, [self.eps_t])
        self.memset("dve", self.one_t.ap, 1.0, [self.one_t])

    def load_featT(self, dst, src1d, n):
        self.dma("sp", dst.ap, src1d.rearrange("(k p) -> p k", p=128), [], [dst], allow_slow_non_contiguous=True)

    def load_w_cast(self, W, dst_c0, src, src_c0, ncols):
        srcv = src.rearrange("(k p) c -> p k c", p=128)
        for c0 in range(0, ncols, 2048):
            n = min(2048, ncols - c0)
            self.dma("pool", W.ap[:, :, dst_c0 + c0:dst_c0 + c0 + n], srcv[:, :, src_c0 + c0:src_c0 + c0 + n], [], [W])

    def load_w_cols(self, W, dst_c0, src, src_c0, ncols, kcn, scaleT, stage, cscale=None):
        srcv = src.rearrange("(k p) c -> p k c", p=128)
        CB = 256
        for i, c0 in enumerate(range(0, ncols, CB)):
            stg = stage[self._stg % 2]
            self._stg += 1
            self.dma("sp", stg.ap[:, 0:kcn, :], srcv[:, :, src_c0 + c0:src_c0 + c0 + CB], [], [stg])
            for kc in range(kcn):
                eng = ("dve", "pool", "act")[kc % 3]
                o = W.ap[:, kc, dst_c0 + c0:dst_c0 + c0 + CB]
                if scaleT is None and cscale is not None:
                    self.ts("dve" if eng == "act" else eng, o, stg.ap[:, kc, :], float(cscale), None, ALU.mult, None, [stg], [W])
                elif scaleT is None:
                    self.cp(eng, o, stg.ap[:, kc, :], [stg], [W])
                elif eng == "act":
                    self.act(o, stg.ap[:, kc, :], AF.Copy, [stg, scaleT], [W], scale=scaleT.ap[:, kc:kc + 1])
                else:
                    self.ts(eng, o, stg.ap[:, kc, :], scaleT.ap[:, kc:kc + 1], None, ALU.mult, None, [stg, scaleT], [W])

    def phase_a1(self):
        ar = self.ar
        T = self.track
        I = self.I
        pb = self.pb
        self._stg = 0
        TT = 512
        W1 = T(ar.alloc((8, 3072), BF16, name="W1"))
        Wua = T(ar.alloc((4, 1024), BF16, name="Wua"))
        stage = [T(ar.alloc((8, 256), F32, name=f"stg{i}")) for i in range(2)]
        g1T = T(ar.alloc(8, F32, name="g1T"))
        self.load_featT(g1T, I["norm1_g"], 8)
        self.load_w_cast(W1, 0, I["w_in"], 0, 2048)
        self.load_w_cast(W1, 2048, I["w_in"], 4096, 1024)
        self.load_w_cast(Wua, 0, I["w_up_a"], 0, 1024)
        hlb0 = T(ar.alloc(4, F32)); hlb1 = T(ar.alloc(4, F32))
        lb = T(ar.alloc(4, F32, name="lb")); oml = T(ar.alloc(4, F32, name="oml")); noml = T(ar.alloc(4, F32, name="noml"))
        gnT = T(ar.alloc(4, F32, name="gnT"))
        self.load_featT(hlb0, I["hg_lower_bounds"][0, :], 4)
        self.load_featT(hlb1, I["hg_lower_bounds"][1, :], 4)
        self.load_featT(gnT, I["hg_norm_g"], 4)
        self.tt("dve", hlb0.ap, hlb0.ap, hlb1.ap, ALU.subtract, [hlb0, hlb1], [hlb0])
        self.act(lb.ap, hlb0.ap, AF.Sigmoid, [hlb0], [lb])
        self.ts("dve", oml.ap, lb.ap, -1.0, 1.0, ALU.mult, ALU.add, [lb], [oml])
        self.ts("dve", noml.ap, oml.ap, -1.0, None, ALU.mult, None, [oml], [noml])
        mask = T(ar.alloc(TT, F32, parts=64, name="mask"))
        self.memset("pool", mask.ap, 1.0, [mask])
        self.S.op("pool", lambda e: e.affine_select(out=mask.ap, in_=mask.ap, pattern=[[0, TT // 64], [1, 64]], compare_op=ALU.is_ge, fill=0.0, base=0, channel_multiplier=-1), [mask], [mask])
        rmask = T(ar.alloc(TT, F32, name="rmask"))
        self.memset("pool", rmask.ap, 1.0, [rmask])
        self.memset("pool", rmask.ap[:, 0::64], 0.0, [rmask])
        xt = [T(ar.alloc(D, F32, name=f"xt{i}")) for i in range(2)]
        xn = [T(ar.alloc(D, BF16, name=f"xn{i}")) for i in range(2)]
        junk = T(ar.alloc(D, BF16, name="junk"))
        ss = [T(ar.alloc(1, F32, name=f"ss{i}")) for i in range(2)]
        hnT_r = [T(ar.alloc((8, TT), BF16, name=f"hnT{i}")) for i in range(2)]
        self._hn = 0
        vt = T(ar.alloc((8, 512), BF16, parts=64, name="vt"))
        HB = []
        for h in range(4):
            d = dict(
                A=T(ar.alloc(TT, F32, name=f"A{h}")), B=T(ar.alloc(TT, F32, name=f"B{h}")), C=T(ar.alloc(TT, F32, name=f"C{h}")),
                qt=T(ar.alloc(TT, BF16, name=f"qt{h}")), kt=T(ar.alloc(TT, BF16, name=f"kt{h}")), kh=T(ar.alloc(TT, BF16, name=f"kh{h}")),
                sm=T(ar.alloc(TT, BF16, parts=64, name=f"sm{h}")), khT=T(ar.alloc((8, 128), BF16, parts=64, name=f"khT{h}")),
                eg=T(ar.alloc(8, F32, name=f"eg{h}")), S=T(ar.alloc(128, F32, name=f"S{h}")),
                Sb=[T(ar.alloc(128, BF16, name=f"Sb{h}{i}")) for i in range(2)], Sm=T(ar.alloc(128, F32, name=f"Sm{h}")),
                oa=T(ar.alloc(TT, BF16, name=f"oa{h}")),
            )
            d["sbi"] = 0
            HB.append(d)
        sga = [T(ar.alloc(TT, BF16, name=f"sga{i}")) for i in range(2)]
        tab = [T(ar.alloc(TT, BF16, name=f"tab{i}")) for i in range(2)]
        print("A1 arena used", ar.off)
        proj = [pb[0], pb[1], pb[7], pb[2], pb[3], pb[4], pb[5], pb[6]]
        self._pj = 0

        def nproj():
            p = proj[self._pj % 8]
            self._pj += 1
            return p
        self._po = 0
        self._ps = 0
        hnTm_v = self.hnTm_d.rearrange("(k p) t -> p k t", p=128)
        hnT_v = self.hnT_d.rearrange("(k p) t -> p k t", p=128)
        ta_v = self.ta_d.rearrange("(k p) t -> p k t", p=128)

        def tile(xrows, Tn, tok0, meta):
            nsub = max(1, Tn // 128)
            tsz = min(Tn, 128)
            CL = min(64, Tn)
            NCH = Tn // CL
            hnT = hnT_r[self._hn % 2]
            self._hn += 1
            for u in range(nsub):
                x_ = xt[u % 2]; xn_ = xn[u % 2]; ss_ = ss[u % 2]
                self.dma("sp", x_.ap[0:tsz, :], xrows[u * tsz:(u + 1) * tsz, :], [], [x_])
                self.act(junk.ap[0:tsz, :], x_.ap[0:tsz, :], AF.Square, [x_], [junk, ss_], accum_out=ss_.ap[0:tsz, :])
                self.act(ss_.ap[0:tsz, :], ss_.ap[0:tsz, :], AF.Ln, [ss_, self.eps_t], [ss_], scale=1.0 / D, bias=self.eps_t.ap[0:tsz, :])
                self.act(ss_.ap[0:tsz, :], ss_.ap[0:tsz, :], AF.Exp, [ss_], [ss_], scale=-0.5)
                self.ts("dve", xn_.ap[0:tsz, :], x_.ap[0:tsz, :], ss_.ap[0:tsz, 0:1], None, ALU.mult, None, [x_, ss_], [xn_])
                P_tr = nproj()
                ptr = P_tr.ap.bitcast(BF16).rearrange("p (k t) -> p k t", k=8)
                for kc in range(8):
                    self.tr(ptr[:, kc, 0:tsz], xn_.ap[0:tsz, kc * 128:(kc + 1) * 128], self.ident_bf.ap[0:tsz, 0:tsz], [xn_, self.ident_bf], [P_tr])
                self.tt("dve", hnT.ap[:, :, u * tsz:(u + 1) * tsz], ptr[:, :, 0:tsz], g1T.ap.unsqueeze(2).to_broadcast([128, 8, tsz]), ALU.mult, [P_tr, g1T], [hnT])
            if meta:
                self.dma_out("sp", hnTm_v, hnT.ap[:, :, 0:Tn], [hnT])
            else:
                self.dma_out("sp", hnT_v[:, :, tok0:tok0 + Tn], hnT.ap[:, :, 0:Tn], [hnT])
            for c in range(NCH):
                p = nproj()
                for kc in range(8):
                    self.mm(p.ap[0:CL, :], hnT.ap[:, kc, c * CL:(c + 1) * CL], W1.ap[:, kc, 1024:1536], kc == 0, kc == 7, [hnT, W1], [p])
                self.cp("act", vt.ap[0:CL, c, :], p.ap[0:CL, :], [p], [vt])
            for h in range(4):
                hb = HB[h]
                A, B, C = hb["A"], hb["B"], hb["C"]
                p = nproj()
                for kc in range(8):
                    self.mm(p.ap[:, 0:Tn], W1.ap[:, kc, 512 + h * 128:512 + (h + 1) * 128], hnT.ap[:, kc, 0:Tn], kc == 0, kc == 7, [hnT, W1], [p])
                self.act(A.ap[:, 0:Tn], p.ap[:, 0:Tn], AF.Sigmoid, [p], [A])
                self.act(B.ap[:, 0:Tn], A.ap[:, 0:Tn], AF.Ln, [A, oml, lb], [B], scale=oml.ap[:, h:h + 1], bias=lb.ap[:, h:h + 1])
                self.ts("dve", A.ap[:, 0:Tn], A.ap[:, 0:Tn], noml.ap[:, h:h + 1], oml.ap[:, h:h + 1], ALU.mult, ALU.add, [A, noml, oml], [A])
                if meta:
                    self.scan(C.ap[:, 0:Tn], self.ones_f.ap[:, 0:Tn], B.ap[:, 0:Tn], 0.0, [B, self.ones_f], [C])
                else:
                    self.scan(C.ap[:, 0:Tn], rmask.ap[:, 0:Tn], B.ap[:, 0:Tn], 0.0, [B, rmask], [C])
                self.act(B.ap[:, 0:Tn], C.ap[:, 0:Tn], AF.Exp, [C], [B])
                self.act(C.ap[:, 0:Tn], C.ap[:, 0:Tn], AF.Exp, [C], [C], scale=-1.0)
                self.cp("pool", hb["eg"].ap[:, 0:NCH], B.ap[:, CL - 1:Tn:CL], [B], [hb["eg"]])
                self.tt("dve", hb["kt"].ap[:, 0:Tn], A.ap[:, 0:Tn], C.ap[:, 0:Tn], ALU.mult, [A, C], [hb["kt"]])
                self.tt("dve", hb["kh"].ap[:, 0:Tn].rearrange("p (c t) -> p c t", c=NCH), hb["kt"].ap[:, 0:Tn].rearrange("p (c t) -> p c t", c=NCH),
                        hb["eg"].ap[:, 0:NCH].unsqueeze(2).to_broadcast([128, NCH, CL]), ALU.mult, [hb["kt"], hb["eg"]], [hb["kh"]])
                P_ktr = nproj()
                pk = P_ktr.ap.bitcast(BF16).rearrange("p (c d) -> p c d", c=8)
                for c in range(NCH):
                    self.tr(pk[0:CL, c, :], hb["kh"].ap[:, c * CL:(c + 1) * CL], self.ident_bf.ap, [hb["kh"], self.ident_bf], [P_ktr])
                self.cp("act", hb["khT"].ap[0:CL, 0:NCH, :], pk[0:CL, 0:NCH, :], [P_ktr], [hb["khT"]])
                if meta:
                    continue
                p = nproj()
                for kc in range(8):
                    self.mm(p.ap[:, 0:Tn], W1.ap[:, kc, h * 128:(h + 1) * 128], hnT.ap[:, kc, 0:Tn], kc == 0, kc == 7, [hnT, W1], [p])
                self.tt("dve", hb["qt"].ap, p.ap, B.ap, ALU.mult, [p, B], [hb["qt"]])
                p = nproj()
                for kc in range(8):
                    self.mm(p.ap[:, 0:Tn], W1.ap[:, kc, 1536 + h * 128:1536 + (h + 1) * 128], hnT.ap[:, kc, 0:Tn], kc == 0, kc == 7, [hnT, W1], [p])
                self.act(B.ap, p.ap, AF.Sigmoid, [p], [B])
                P_sc = nproj()
                for c in range(NCH):
                    self.mm(P_sc.ap[0:64, c * 64:(c + 1) * 64], hb["kt"].ap[:, c * 64:(c + 1) * 64], hb["qt"].ap[:, c * 64:(c + 1) * 64], True, True, [hb["kt"], hb["qt"]], [P_sc])
                self.tt("dve", hb["sm"].ap, P_sc.ap[0:64, :], mask.ap, ALU.mult, [P_sc, mask], [hb["sm"]])
            for c in range(NCH):
                for h in range(4):
                    hb = HB[h]
                    hs = slice(h * 128, (h + 1) * 128)
                    Sb = hb["Sb"][hb["sbi"] % 2]
                    if not meta:
                        pob = nproj()
                        po = pob.ap[:, 0:64]
                        self.mm(po, vt.ap[0:64, c, hs], hb["sm"].ap[0:64, c * 64:(c + 1) * 64], True, False, [vt, hb["sm"]], [pob])
                        self.mm(po, Sb.ap, hb["qt"].ap[:, c * 64:(c + 1) * 64], False, True, [Sb, hb["qt"]], [pob])
                        self.cp("act", hb["A"].ap[:, c * 64:(c + 1) * 64], po, [pob], [hb["A"]])
                    psb = nproj()
                    psl = psb.ap[:, 0:128]
                    self.mm(psl, hb["khT"].ap[0:CL, c, :], vt.ap[0:CL, c, hs], True, True, [hb["khT"], vt], [psb])
                    if meta:
                        self.cp("dve", hb["Sm"].ap, psl, [psb], [hb["Sm"]])
                    else:
                        self.stt(hb["S"].ap, hb["S"].ap, hb["eg"].ap[:, c:c + 1], psl, ALU.mult, ALU.add, [hb["S"], hb["eg"], psb], [hb["S"]])
                        hb["sbi"] += 1
                        Sb2 = hb["Sb"][hb["sbi"] % 2]
                        self.cp("act", Sb2.ap, hb["S"].ap, [hb["S"]], [Sb2])
            if meta:
                return
            for h in range(4):
                hb = HB[h]
                A, B, C = hb["A"], hb["B"], hb["C"]
                self.act(C.ap, A.ap, AF.Square, [A], [C])
                p = nproj()
                self.mm(p.ap, self.ones_f.ap, C.ap, True, True, [self.ones_f, C], [p])
                self.act(C.ap, p.ap, AF.Ln, [p, self.eps_t], [C], scale=1.0 / 128, bias=self.eps_t.ap)
                self.act(C.ap, C.ap, AF.Exp, [C], [C], scale=-0.5)
                self.stt(A.ap, A.ap, gnT.ap[:, h:h + 1], C.ap, ALU.mult, ALU.mult, [A, gnT, C], [A])
                self.tt("dve", hb["oa"].ap, A.ap, B.ap, ALU.mult, [A, B], [hb["oa"]])
            for mc in range(8):
                p = nproj()
                for kc in range(8):
                    self.mm(p.ap, W1.ap[:, kc, 2048 + mc * 128:2048 + (mc + 1) * 128], hnT.ap[:, kc, :], kc == 0, kc == 7, [hnT, W1], [p])
                sg = sga[mc % 2]
                self.act(sg.ap, p.ap, AF.Sigmoid, [p], [sg])
                p2 = nproj()
                for h in range(4):
                    self.mm(p2.ap, Wua.ap[:, h, mc * 128:(mc + 1) * 128], HB[h]["oa"].ap, h == 0, h == 3, [Wua, HB[h]["oa"]], [p2])
                tb_ = tab[mc % 2]
                self.tt("dve", tb_.ap, p2.ap, sg.ap, ALU.mult, [p2, sg], [tb_])
                self.dma_out("sp", ta_v[:, mc, tok0:tok0 + Tn], tb_.ap, [tb_])

        if self.stop == "a1_setup":
            return
        tile(I["meta_tokens"], NMETA, None, True)
        if self.stop == "a1_meta":
            return
        for s in range(self.nseq):
            for h in range(4):
                hb = HB[h]
                self.cp("dve", hb["S"].ap, hb["Sm"].ap, [hb["Sm"]], [hb["S"]])
                hb["sbi"] += 1
                self.cp("act", hb["Sb"][hb["sbi"] % 2].ap, hb["Sm"].ap, [hb["Sm"]], [hb["Sb"][hb["sbi"] % 2]])
            for j in range(SEQ // TT):
                tok0 = s * SEQ + j * TT
                tile(I["x"][tok0:tok0 + TT, :], TT, tok0, False)

    def phase_a2(self):
        ar = self.ar
        T = self.track
        I = self.I
        pb = self.pb
        TT = 512
        NTL = self.ntok // 128
        W2 = T(ar.alloc((8, 3072), BF16, name="W2"))
        Wub = T(ar.alloc((8, 1024), BF16, name="Wub"))
        Wout = T(ar.alloc((8, 1024), BF16, name="Wout"))
        wr = T(ar.alloc((8, 128), BF16, name="wr")); wi = T(ar.alloc((8, 128), BF16, name="wi"))
        stage = [T(ar.alloc((8, 256), F32, name=f"stg{i}")) for i in range(2)]
        g1T = T(ar.alloc(8, F32, name="g1T")); g2T = T(ar.alloc(8, F32, name="g2T"))
        self.load_featT(g1T, I["norm1_g"], 8)
        self.load_featT(g2T, I["norm2_g"], 8)
        self.load_w_cast(W2, 0, I["w_in"], 2048, 2048)
        self.load_w_cast(W2, 2048, I["w_in"], 5120, 1024)
        self.load_w_cast(Wub, 0, I["w_up_b"], 0, 1024)
        self.load_w_cast(Wout, 0, I["w_out"], 0, 1024)
        for (w_, src) in ((wr, I["lru_w_r"]), (wi, I["lru_w_i"])):
            self.dma("pool", w_.ap, src.rearrange("n i j -> i n j"), [], [w_])
        cwT = T(ar.alloc((4, 8), F32, name="cwT"))
        for j in range(4):
            self.dma("sp", cwT.ap[:, j, :], I["conv_w"][j, :].rearrange("(k p) -> p k", p=128), [], [cwT], allow_slow_non_contiguous=True)
        cbT = T(ar.alloc(8, F32)); brT = T(ar.alloc(8, F32)); biT = T(ar.alloc(8, F32)); lamT = T(ar.alloc(8, F32))
        cl = T(ar.alloc(8, F32)); cl2 = T(ar.alloc(8, F32))
        self.load_featT(cbT, I["conv_b"], 8)
        self.load_featT(brT, I["lru_b_r"], 8)
        self.load_featT(biT, I["lru_b_i"], 8)
        self.load_featT(lamT, I["lru_lambda"], 8)
        self.act(lamT.ap, lamT.ap, AF.Exp, [lamT], [lamT], scale=-1.0)
        self.act(lamT.ap, lamT.ap, AF.Ln, [lamT, self.one_t], [lamT], bias=self.one_t.ap)
        self.ts("dve", cl.ap, lamT.ap, -4.0, None, ALU.mult, None, [lamT], [cl])
        self.ts("dve", cl2.ap, lamT.ap, -8.0, None, ALU.mult, None, [lamT], [cl2])
        self.ts("dve", brT.ap, brT.ap, 0.5, None, ALU.mult, None, [brT], [brT])
        self.ts("dve", biT.ap, biT.ap, 0.5, None, ALU.mult, None, [biT], [biT])
        lnh = T(ar.alloc(1, F32, name="lnh"))
        self.memset("dve", lnh.ap, -0.6931471805599453, [lnh])
        g2bc = T(ar.alloc(D, F32, name="g2bc"))
        self.dma("sp", g2bc.ap, I["norm2_g"].partition_broadcast(128), [], [g2bc])
        Wr = T(ar.alloc((8, 36), F32, name="Wr"))
        self.dma("sp", Wr.ap[:, :, 0:4], I["w_group"].rearrange("(k p) c -> p k c", p=128), [], [Wr])
        for g in range(4):
            self.dma("sp", Wr.ap[:, :, 4 + g * 8:12 + g * 8], I["w_router"][g].rearrange("(k p) c -> p k c", p=128), [], [Wr])
        for kc in range(8):
            self.ts("dve", Wr.ap[:, kc, :], Wr.ap[:, kc, :], g2T.ap[:, kc:kc + 1], None, ALU.mult, None, [Wr, g2T], [Wr])
        brt = T(ar.alloc(36, F32, name="brt"))
        self.dma("sp", brt.ap[:, 0:4], I["b_group"].partition_broadcast(128), [], [brt])
        self.dma("sp", brt.ap[:, 4:36], I["b_router"].partition_broadcast(128), [], [brt])
        hist = [T(ar.alloc(3, F32, name=f"hist{c}")) for c in range(8)]
        histm = [T(ar.alloc(3, F32, name=f"histm{c}")) for c in range(8)]
        hst = [T(ar.alloc(1, F32, name=f"hst{c}")) for c in range(8)]
        hstm = [T(ar.alloc(1, F32, name=f"hstm{c}")) for c in range(8)]
        for c in range(8):
            self.memset("dve", hist[c].ap, 0.0, [hist[c]])
            self.memset("dve", hst[c].ap, 0.0, [hst[c]])
        hnT = T(ar.alloc((8, TT), BF16, name="hnT"))
        lxb = [T(ar.alloc(TT + 3, F32, name=f"lxb{i}")) for i in range(2)]
        NR = 2
        xc = [T(ar.alloc(TT, F32, name=f"xc{i}")) for i in range(NR)]
        xcb = [T(ar.alloc(TT, BF16, name=f"xcb{i}")) for i in range(NR)]
        Rb = [T(ar.alloc(TT, F32, name=f"R{i}")) for i in range(NR)]
        Ib = [T(ar.alloc(TT, F32, name=f"I{i}")) for i in range(NR)]
        Ab = [T(ar.alloc(TT, F32, name=f"Ab{i}")) for i in range(NR)]
        Hb = [T(ar.alloc(TT, F32, name=f"H{i}")) for i in range(NR)]
        ob = [T(ar.alloc(TT, BF16, name=f"ob{c}")) for c in range(8)]
        tat = T(ar.alloc((8, TT), BF16, name="tat"))
        sgb = [T(ar.alloc(TT, BF16, name=f"sgb{i}")) for i in range(2)]
        t2 = [T(ar.alloc(TT, F32, name=f"t2{i}")) for i in range(2)]
        mg = [T(ar.alloc(TT, BF16, name=f"mg{c}")) for c in range(8)]
        xr = [T(ar.alloc(D, F32, name=f"xr{i}")) for i in range(2)]
        hn2t = [T(ar.alloc(D, BF16, name=f"hn2t{i}")) for i in range(2)]
        h2T = T(ar.alloc((8, 128), F32, name="h2T"))
        ss = [T(ar.alloc(1, F32, name=f"ss2{i}")) for i in range(2)]
        rt = [dict(lg=T(ar.alloc(36, F32)), gmax=T(ar.alloc(1, F32)), gm=T(ar.alloc(4, F32)), ge=T(ar.alloc(4, F32)), gsum=T(ar.alloc(1, F32)),
                   pen=T(ar.alloc(4, F32)), ml=T(ar.alloc(32, F32)), top=T(ar.alloc(8, F32)), d=T(ar.alloc(1, F32))) for i in range(2)]
        print("A2 arena used", ar.off)
        proj = [pb[0], pb[1], pb[7], pb[5], pb[6], pb[2], pb[3], pb[4]]
        self._pj = 0

        def nproj():
            p = proj[self._pj % 8]
            self._pj += 1
            return p
        hnTm_v = self.hnTm_d.rearrange("(k p) t -> p k t", p=128)
        hnT_v = self.hnT_d.rearrange("(k p) t -> p k t", p=128)
        ta_v = self.ta_d.rearrange("(k p) t -> p k t", p=128)
        Mall, wts = self.Mall, self.wts
        BIG = 1.0e30

        def tile(Tn, tok0, meta):
            if meta:
                self.dma("sp", hnT.ap[:, :, 0:Tn], hnTm_v, [], [hnT])
            else:
                self.dma("sp", hnT.ap[:, :, 0:Tn], hnT_v[:, :, tok0:tok0 + Tn], [], [hnT])
                self.dma("sp", tat.ap, ta_v[:, :, tok0:tok0 + Tn], [], [tat])
            for c in range(8):
                i = c % NR
                lx_ = lxb[c % 2]; xc_ = xc[i]; xcb_ = xcb[i]; R_ = Rb[i]; I_ = Ib[i]; A_ = Ab[i]; H_ = Hb[i]
                p = nproj()
                for kc in range(8):
                    self.mm(p.ap[:, 0:Tn], W2.ap[:, kc, c * 128:(c + 1) * 128], hnT.ap[:, kc, 0:Tn], kc == 0, kc == 7, [hnT, W2], [p])
                self.cp("pool", lx_.ap[:, 0:3], hist[c].ap, [hist[c]], [lx_])
                self.cp("act", lx_.ap[:, 3:3 + Tn], p.ap[:, 0:Tn], [p], [lx_])
                self.ts("dve", xc_.ap[:, 0:Tn], lx_.ap[:, 0:Tn], cwT.ap[:, 0, c:c + 1], cbT.ap[:, c:c + 1], ALU.mult, ALU.add, [lx_, cwT, cbT], [xc_])
                for j in range(1, 4):
                    self.stt(xc_.ap[:, 0:Tn], lx_.ap[:, j:j + Tn], cwT.ap[:, j, c:c + 1], xc_.ap[:, 0:Tn], ALU.mult, ALU.add, [lx_, cwT, xc_], [xc_])
                self.cp("pool", hist[c].ap, lx_.ap[:, Tn:Tn + 3], [lx_], [hist[c]])
                self.cp("pool", xcb_.ap[:, 0:Tn], xc_.ap[:, 0:Tn], [xc_], [xcb_])
                P_r = nproj(); P_i = nproj()
                self.mm(P_r.ap[:, 0:Tn], wr.ap[:, c, :], xcb_.ap[:, 0:Tn], True, True, [wr, xcb_], [P_r])
                self.mm(P_i.ap[:, 0:Tn], wi.ap[:, c, :], xcb_.ap[:, 0:Tn], True, True, [wi, xcb_], [P_i])
                self.act(R_.ap[:, 0:Tn], P_r.ap[:, 0:Tn], AF.Tanh, [P_r, brT], [R_], bias=brT.ap[:, c:c + 1], scale=0.5)
                self.act(I_.ap[:, 0:Tn], P_i.ap[:, 0:Tn], AF.Tanh, [P_i, biT], [I_], bias=biT.ap[:, c:c + 1], scale=0.5)
                self.act(A_.ap[:, 0:Tn], R_.ap[:, 0:Tn], AF.Exp, [R_, cl], [A_], scale=cl.ap[:, c:c + 1], bias=cl.ap[:, c:c + 1])
                self.act(R_.ap[:, 0:Tn], R_.ap[:, 0:Tn], AF.Exp, [R_, cl2], [R_], scale=cl2.ap[:, c:c + 1], bias=cl2.ap[:, c:c + 1])
                self.act(R_.ap[:, 0:Tn], R_.ap[:, 0:Tn], AF.Ln, [R_, self.one_t], [R_], scale=-1.0, bias=self.one_t.ap)
                self.act(R_.ap[:, 0:Tn], R_.ap[:, 0:Tn], AF.Exp, [R_, lnh], [R_], scale=0.5, bias=lnh.ap)
                self.stt(I_.ap[:, 0:Tn], I_.ap[:, 0:Tn], 1.0, xc_.ap[:, 0:Tn], ALU.add, ALU.mult, [I_, xc_], [I_])
                self.tt("dve", I_.ap[:, 0:Tn], I_.ap[:, 0:Tn], R_.ap[:, 0:Tn], ALU.mult, [I_, R_], [I_])
                self.scan(H_.ap[:, 0:Tn], A_.ap[:, 0:Tn], I_.ap[:, 0:Tn], hst[c].ap[:, 0:1], [A_, I_, hst[c]], [H_])
                self.cp("pool", hst[c].ap, H_.ap[:, Tn - 1:Tn], [H_], [hst[c]])
                if meta:
                    self.cp("pool", hstm[c].ap, hst[c].ap, [hst[c]], [hstm[c]])
                    self.cp("pool", histm[c].ap, hist[c].ap, [hist[c]], [histm[c]])
                    continue
                p = nproj()
                for kc in range(8):
                    self.mm(p.ap, W2.ap[:, kc, 1024 + c * 128:1024 + (c + 1) * 128], hnT.ap[:, kc, :], kc == 0, kc == 7, [hnT, W2], [p])
                self.act(R_.ap, p.ap, AF.Gelu_apprx_tanh, [p], [R_])
                self.stt(ob[c].ap, H_.ap, 0.5, R_.ap, ALU.mult, ALU.mult, [H_, R_], [ob[c]])
            if meta:
                return
            for mc in range(8):
                p = nproj()
                for kc in range(8):
                    self.mm(p.ap, W2.ap[:, kc, 2048 + mc * 128:2048 + (mc + 1) * 128], hnT.ap[:, kc, :], kc == 0, kc == 7, [hnT, W2], [p])
                sg = sgb[mc % 2]; t2_ = t2[mc % 2]
                self.act(sg.ap, p.ap, AF.Tanh, [p], [sg], scale=0.5)
                P_u = nproj()
                for c in range(8):
                    self.mm(P_u.ap, Wub.ap[:, c, mc * 128:(mc + 1) * 128], ob[c].ap, c == 0, c == 7, [Wub, ob[c]], [P_u])
                self.stt(t2_.ap, sg.ap, 1.0, P_u.ap, ALU.add, ALU.mult, [P_u, sg], [t2_])
                self.tt("pool", mg[mc].ap, t2_.ap, tat.ap[:, mc, :], ALU.add, [t2_, tat], [mg[mc]])
            for u in range(Tn // 128):
                t0 = tok0 + u * 128
                til = t0 // 128
                xr_ = xr[u % 2]; ss_ = ss[u % 2]; hn2_ = hn2t[u % 2]; r_ = rt[u % 2]
                self.dma("sp", xr_.ap, I["x"][t0:t0 + 128, :], [], [xr_])
                for hf in range(2):
                    p = nproj()
                    for mc in range(8):
                        self.mm(p.ap, mg[mc].ap[:, u * 128:(u + 1) * 128], Wout.ap[:, mc, hf * 512:(hf + 1) * 512], mc == 0, mc == 7, [mg[mc], Wout], [p])
                    self.tt("dve", xr_.ap[:, hf * 512:(hf + 1) * 512], xr_.ap[:, hf * 512:(hf + 1) * 512], p.ap, ALU.add, [xr_, p], [xr_])
                self.dma_out("sp", self.h2_d[t0:t0 + 128, :], xr_.ap, [xr_])
                self.act(hn2_.ap, xr_.ap, AF.Square, [xr_], [hn2_, ss_], accum_out=ss_.ap)
                self.act(ss_.ap, ss_.ap, AF.Ln, [ss_, self.eps_t], [ss_], scale=1.0 / D, bias=self.eps_t.ap)
                self.act(ss_.ap, ss_.ap, AF.Exp, [ss_], [ss_], scale=-0.5)
                self.stt(hn2_.ap, xr_.ap, ss_.ap[:, 0:1], g2bc.ap, ALU.mult, ALU.mult, [xr_, ss_, g2bc], [hn2_])
                self.dma_out("sp", self.hn2_d[t0:t0 + 128, :], hn2_.ap, [hn2_])
                P_t0 = nproj(); P_t1 = nproj()
                for kc in range(8):
                    P_t = P_t0 if kc < 4 else P_t1
                    self.tr(P_t.ap[:, (kc % 4) * 128:(kc % 4 + 1) * 128], xr_.ap[:, kc * 128:(kc + 1) * 128], self.ident_f.ap, [xr_, self.ident_f], [P_t])
                self.cp("act", h2T.ap[:, 0:4, :], P_t0.ap.rearrange("p (k t) -> p k t", k=4), [P_t0], [h2T])
                self.cp("dve", h2T.ap[:, 4:8, :], P_t1.ap.rearrange("p (k t) -> p k t", k=4), [P_t1], [h2T])
                p = nproj()
                for kc in range(8):
                    self.mm(p.ap[:, 0:36], h2T.ap[:, kc, :], Wr.ap[:, kc, :], kc == 0, kc == 7, [h2T, Wr], [p])
                lg = r_["lg"]
                self.stt(lg.ap, p.ap[:, 0:36], ss_.ap[:, 0:1], brt.ap, ALU.mult, ALU.add, [p, ss_, brt], [lg])
                self.S.op("dve", lambda e, o=r_["gmax"].ap, i=lg.ap[:, 0:4]: e.tensor_reduce(out=o, in_=i, axis=AX.X, op=ALU.max), [lg], [r_["gmax"]])
                self.ts("dve", r_["gm"].ap, lg.ap[:, 0:4], r_["gmax"].ap[:, 0:1], None, ALU.is_equal, None, [lg, r_["gmax"]], [r_["gm"]])
                self.ts("dve", r_["pen"].ap, r_["gm"].ap, BIG, -BIG, ALU.mult, ALU.add, [r_["gm"]], [r_["pen"]])
                self.ts("dve", r_["gmax"].ap, r_["gmax"].ap, -1.0, None, ALU.mult, None, [r_["gmax"]], [r_["gmax"]])
                self.act(r_["ge"].ap, lg.ap[:, 0:4], AF.Exp, [lg, r_["gmax"]], [r_["ge"], r_["gsum"]], bias=r_["gmax"].ap[:, 0:1], accum_out=r_["gsum"].ap)
                self.recip(r_["gsum"].ap, r_["gsum"].ap, [r_["gsum"]], [r_["gsum"]])
                self.tt("dve", r_["ml"].ap.rearrange("p (g e) -> p g e", g=4), lg.ap[:, 4:36].rearrange("p (g e) -> p g e", g=4),
                        r_["pen"].ap.unsqueeze(2).to_broadcast([128, 4, 8]), ALU.add, [lg, r_["pen"]], [r_["ml"]])
                self.S.op("dve", lambda e, o=r_["top"].ap, i=r_["ml"].ap: e.max(out=o, in_=i), [r_["ml"]], [r_["top"]])
                self.ts("dve", Mall.ap[:, til, 0:32], r_["ml"].ap, r_["top"].ap[:, 0:1], None, ALU.is_equal, None, [r_["ml"], r_["top"]], [Mall])
                self.ts("dve", Mall.ap[:, til, 32:64], r_["ml"].ap, r_["top"].ap[:, 1:2], None, ALU.is_equal, None, [r_["ml"], r_["top"]], [Mall])
                self.tt("dve", r_["d"].ap, r_["top"].ap[:, 1:2], r_["top"].ap[:, 0:1], ALU.subtract, [r_["top"]], [r_["d"]])
                self.act(r_["d"].ap, r_["d"].ap, AF.Exp, [r_["d"]], [r_["d"]])
                self.ts("dve", r_["d"].ap, r_["d"].ap, 1.0, None, ALU.add, None, [r_["d"]], [r_["d"]])
                self.recip(r_["d"].ap, r_["d"].ap, [r_["d"]], [r_["d"]])
                self.tt("dve", wts.ap[:, til, 0:1], r_["d"].ap, r_["gsum"].ap, ALU.mult, [r_["d"], r_["gsum"]], [wts])
                self.tt("dve", wts.ap[:, til, 1:2], r_["gsum"].ap, wts.ap[:, til, 0:1], ALU.subtract, [r_["gsum"], wts], [wts])

        tile(NMETA, None, True)
        for s_ in range(self.nseq):
            for c in range(8):
                self.cp("pool", hst[c].ap, hstm[c].ap, [hstm[c]], [hst[c]])
                self.cp("pool", hist[c].ap, histm[c].ap, [histm[c]], [hist[c]])
            for j in range(SEQ // TT):
                tile(TT, s_ * SEQ + j * TT, False)


    def phase_b0(self):
        ar = self.ar
        T = self.track
        pb = self.pb
        NTL = self.ntok // 128
        NB = self.NB
        NTk = self.ntok
        Mall, wts, bei = self.Mall, self.wts, self.bei
        W = NTL * 32
        Msum = T(ar.alloc((NTL, 32), BF16, name="Msum"))
        U = T(ar.alloc(128, BF16, name="U")); onesb = T(ar.alloc(128, BF16, name="onesb"))
        Uf = T(ar.alloc(128, F32, name="Uf"))
        cs = T(ar.alloc((NTL, 32), F32, name="cs")); rk = T(ar.alloc((NTL, 32), F32, name="rk"))
        pre = T(ar.alloc((NTL + 1, 32), F32, name="pre"))
        tmp = T(ar.alloc((NTL, 32), F32, name="tmp"))
        cnt = T(ar.alloc(32, F32)); cnti = T(ar.alloc(32, I32)); pcnt = T(ar.alloc(32, F32)); pend = T(ar.alloc(32, F32)); pst = T(ar.alloc(32, F32))
        dst = T(ar.alloc((NTL, 2), F32, name="dst")); dsti = self.dsti
        tokI = T(ar.alloc(NTL, I32, name="tokI"))
        row = T(ar.alloc((NTL, 2, 4), I32, name="row"))
        NSR = NB * BLK // 128
        sinit = T(ar.alloc((NSR, 4), I32, name="sinit"))
        bvi = T(ar.alloc(NB, I32)); bv = T(ar.alloc(NB, F32)); cmp_ = T(ar.alloc((NB, 32), F32, name="cmp")); bef = T(ar.alloc(NB, F32))
        print("B0 arena used", ar.off)
        self.tt("dve", Msum.ap, Mall.ap[:, :, 0:32], Mall.ap[:, :, 32:64], ALU.add, [Mall], [Msum])
        self.memset("pool", Uf.ap, 1.0, [Uf])
        self.S.op("pool", lambda e: e.affine_select(out=Uf.ap, in_=Uf.ap, pattern=[[1, 128]], compare_op=ALU.is_gt, fill=0.0, base=0, channel_multiplier=-1), [Uf], [Uf])
        self.cp("dve", U.ap, Uf.ap, [Uf], [U])
        self.memset("dve", onesb.ap, 1.0, [onesb])
        Mf = Msum.ap.rearrange("p t e -> p (t e)")
        csf = cs.ap.rearrange("p t e -> p (t e)")
        rkf = rk.ap.rearrange("p t e -> p (t e)")
        for c0 in range(0, W, 512):
            n = min(512, W - c0)
            self.mm(pb[0].ap[:, 0:n], onesb.ap, Mf[:, c0:c0 + n], True, True, [onesb, Msum], [pb[0]])
            self.cp("act", csf[:, c0:c0 + n], pb[0].ap[:, 0:n], [pb[0]], [cs])
            self.mm(pb[1].ap[:, 0:n], U.ap, Mf[:, c0:c0 + n], True, True, [U, Msum], [pb[1]])
            self.cp("dve", rkf[:, c0:c0 + n], pb[1].ap[:, 0:n], [pb[1]], [rk])
        self.memset("dve", pre.ap[:, 0, :], 0.0, [pre])
        for t in range(NTL):
            self.tt("dve", pre.ap[:, t + 1, :], pre.ap[:, t, :], cs.ap[:, t, :], ALU.add, [pre, cs], [pre])
        self.ts("dve", cnti.ap, pre.ap[:, NTL, :], float(BLK - 1), None, ALU.add, None, [pre], [cnti])
        self.ts("dve", cnti.ap, cnti.ap, 9, 9, ALU.arith_shift_right, ALU.logical_shift_left, [cnti], [cnti])
        self.cp("dve", pcnt.ap, cnti.ap, [cnti], [pcnt])
        self.scan(pend.ap, self.ones_f.ap[:, 0:32], pcnt.ap, 0.0, [pcnt, self.ones_f], [pend])
        self.tt("dve", pst.ap, pend.ap, pcnt.ap, ALU.subtract, [pend, pcnt], [pst])
        self.tt("dve", rk.ap, rk.ap, pre.ap[:, 0:NTL, :], ALU.add, [rk, pre], [rk])
        self.tt("dve", rk.ap, rk.ap, pst.ap.unsqueeze(1).to_broadcast([128, NTL, 32]), ALU.add, [rk, pst], [rk])
        for k in range(2):
            self.tt("dve", tmp.ap, Mall.ap[:, :, k * 32:(k + 1) * 32], rk.ap, ALU.mult, [Mall, rk], [tmp])
            self.S.op("dve", lambda e, o=dst.ap[:, :, k], i=tmp.ap: e.tensor_reduce(out=o, in_=i, axis=AX.X, op=ALU.add), [tmp], [dst])
        self.cp("dve", dsti.ap, dst.ap, [dst], [dsti])
        self.S.op("pool", lambda e: e.iota(tokI.ap, pattern=[[128, NTL]], base=0, channel_multiplier=1), [], [tokI])
        self.memset("dve", row.ap, 0, [row])
        for k in range(2):
            self.cp("dve", row.ap[:, :, k, 0], tokI.ap, [tokI], [row])
            self.ts("dve", row.ap[:, :, k, 1], tokI.ap, float(k * NTk), None, ALU.add, None, [tokI], [row])
            self.cp("dve", row.ap.bitcast(F32)[:, :, k, 2], wts.ap[:, :, k], [wts], [row])
        self.memset("pool", sinit.ap, 0, [sinit])
        self.memset("pool", sinit.ap[:, :, 1:2], 2 * NTk, [sinit])
        sl_init = T(Tl(None, "slot_init"))
        self.dma("sp", self.slot_d.rearrange("(p a) f -> p a f", p=128), sinit.ap, [sinit], [sl_init])
        for t in range(NTL):
            for k in range(2):
                tb = T(Tl(None, "slot_sc"))
                self.idma(lambda e, t=t, k=k: e.indirect_dma_start(out=self.slot_d[:, :], out_offset=bass.IndirectOffsetOnAxis(ap=dsti.ap[:, t, k:k + 1], axis=0),
                                                                  in_=row.ap[:, t, k, :], in_offset=None), 2048, [dsti, row, sl_init], [tb])
        self.S.op("pool", lambda e: e.iota(bvi.ap, pattern=[[BLK, NB]], base=0, channel_multiplier=0), [], [bvi])
        self.cp("dve", bv.ap, bvi.ap, [bvi], [bv])
        self.tt("dve", cmp_.ap, pend.ap.unsqueeze(1).to_broadcast([128, NB, 32]), bv.ap.unsqueeze(2).to_broadcast([128, NB, 32]), ALU.is_le, [pend, bv], [cmp_])
        self.S.op("dve", lambda e: e.tensor_reduce(out=bef.ap, in_=cmp_.ap, axis=AX.X, op=ALU.add), [cmp_], [bef])
        flg = T(ar.alloc(NB, F32, name="flg"))
        self.ts("dve", flg.ap, bef.ap, float(NE) - 0.5, 16384.0, ALU.is_gt, ALU.mult, [bef], [flg])
        self.ts("dve", bef.ap, bef.ap, float(NE - 1), None, ALU.min, None, [bef], [bef])
        pI = T(ar.alloc(NB, I32, name="pI")); pF = T(ar.alloc(NB, F32, name="pF"))
        self.S.op("pool", lambda e: e.iota(pI.ap, pattern=[[0, NB]], base=0, channel_multiplier=1), [], [pI])
        self.cp("dve", pF.ap, pI.ap, [pI], [pF])
        self.stt(bef.ap, bef.ap, 128.0, pF.ap, ALU.mult, ALU.add, [bef, pF], [bef])
        self.stt(bef.ap, bef.ap, 2.0, flg.ap, ALU.mult, ALU.add, [bef, flg], [bef])
        self.cp("dve", bei.ap[:, :, 0], bef.ap, [bef], [bei])
        self.ts("dve", bef.ap, bef.ap, 1.0, None, ALU.add, None, [bef], [bef])
        self.cp("dve", bei.ap[:, :, 1], bef.ap, [bef], [bei])

    def phase_b(self):
        ar = self.ar
        T = self.track
        I = self.I
        pb = self.pb
        NB = self.NB
        NTk = self.ntok
        bei = self.bei
        wg = [T(ar.alloc((8, 512), BF16, name=f"wg{i}")) for i in range(3)]
        wu = [T(ar.alloc((8, 512), BF16, name=f"wu{i}")) for i in range(3)]
        wd = [T(ar.alloc((4, 1024), BF16, name=f"wd{i}")) for i in range(3)]
        xg = [T(ar.alloc(D, BF16, name=f"xg{i}")) for i in range(8)]
        xT = [T(ar.alloc((8, 512), BF16, name=f"xT{i}")) for i in range(3)]
        sgt = [T(ar.alloc(512, F32, name=f"sgt{i}")) for i in range(2)]
        hT = [T(ar.alloc((4, 512), BF16, name=f"hT{i}")) for i in range(3)]
        ysb = [T(ar.alloc(D, F32, name=f"ysb{i}")) for i in range(4)]
        sl = [T(ar.alloc((4, 4), I32, name=f"sl{i}")) for i in range(3)]
        print("B arena used", ar.off)
        wgv = I["w_gate"].rearrange("e (p h k) f -> (e p h) (k f)", p=128, h=2)
        wuv = I["w_up"].rearrange("e (p h k) f -> (e p h) (k f)", p=128, h=2)
        wdv = I["w_down"].rearrange("e (p h k) f -> (e p h) (k f)", p=128, h=2)
        ring = [pb[0], pb[1], pb[7], pb[4], pb[5], pb[6], pb[2], pb[3]]
        cnt = dict(r=0, xg=0, y=0)

        def nbank():
            p = ring[cnt["r"] % 8]
            cnt["r"] += 1
            return p
        regs = {}

        def wload(b, dst, view):
            d2 = dst.ap.rearrange("p k f -> p (k f)")
            for half in range(2):
                def wl(e, o=d2[:, half * 2048:(half + 1) * 2048], src=view, ix=bei.ap[:, b, half:half + 1]):
                    if "wb" not in regs:
                        regs["wb"] = e.to_reg(NE * 256 - 1)
                    return e.indirect_dma_start(out=o, out_offset=None, in_=src, in_offset=bass.IndirectOffsetOnAxis(ap=ix, axis=0),
                                                bounds_check=regs["wb"], oob_is_err=False)
                self.idma(wl, 1 << 20, [bei], [dst])

        for b in range(NB):
            bb = b % 3
            sl_ = sl[bb]
            self.dma("sp", sl_.ap, self.slot_d[b * BLK:(b + 1) * BLK, :].rearrange("(s p) f -> p s f", p=128), [], [sl_])
            wload(b, wg[bb], wgv)
            wload(b, wu[bb], wuv)
            wload(b, wd[bb], wdv)
            xT_ = xT[bb]; hT_ = hT[bb]
            for s_ in range(4):
                xg_ = xg[cnt["xg"] % 8]; cnt["xg"] += 1
                self.idma(lambda e, o=xg_.ap, ix=sl_.ap[:, s_, 0:1]: e.indirect_dma_start(out=o, out_offset=None, in_=self.hn2_d[:, :],
                          in_offset=bass.IndirectOffsetOnAxis(ap=ix, axis=0)), 1 << 18, [sl_], [xg_])
                pt = nbank()
                ptv = pt.ap.bitcast(BF16).rearrange("p (k t) -> p k t", k=8)
                for kc in range(8):
                    self.tr(ptv[:, kc, :], xg_.ap[:, kc::8], self.ident_bf.ap, [xg_, self.ident_bf], [pt])
                self.cp("act" if s_ % 2 == 0 else "dve", xT_.ap[:, :, s_ * 128:(s_ + 1) * 128], ptv, [pt], [xT_])
            for fc in range(4):
                pg = nbank()
                pu = nbank()
                for kc in range(8):
                    self.mm(pg.ap, wg[bb].ap[:, kc, fc::4], xT_.ap[:, kc, :], kc == 0, kc == 7, [wg[bb], xT_], [pg])
                for kc in range(8):
                    self.mm(pu.ap, wu[bb].ap[:, kc, fc::4], xT_.ap[:, kc, :], kc == 0, kc == 7, [wu[bb], xT_], [pu])
                sg_ = sgt[fc % 2]
                self.act(sg_.ap, pg.ap, AF.Silu, [pg], [sg_])
                self.tt("dve", hT_.ap[:, fc, :], pu.ap, sg_.ap, ALU.mult, [pu, sg_], [hT_])
            for s_ in range(4):
                y_ = ysb[cnt["y"] % 4]; cnt["y"] += 1
                wsl = sl_.ap.bitcast(F32)[:, s_, 2:3]
                for hf in range(2):
                    p = nbank()
                    for fc in range(4):
                        self.mm(p.ap, hT_.ap[:, fc, s_ * 128:(s_ + 1) * 128], wd[bb].ap[:, fc, hf * 512:(hf + 1) * 512], fc == 0, fc == 3, [hT_, wd[bb]], [p])
                    if hf == 0:
                        self.act(y_.ap[:, 0:512], p.ap, AF.Copy, [p, sl_], [y_], scale=wsl)
                    else:
                        self.ts("dve", y_.ap[:, 512:1024], p.ap, wsl, None, ALU.mult, None, [p, sl_], [y_])
                r0 = b * BLK + s_ * 128
                self.dma_out("sp", self.Y_d[r0:r0 + 128, :], y_.ap, [y_])

    def phase_c(self):
        ar = self.ar
        T = self.track
        I = self.I
        NTL = self.ntok // 128
        NTk = self.ntok
        fgbc = T(ar.alloc(D, F32, name="fgbc"))
        self.dma("sp", fgbc.ap, I["final_g"].partition_broadcast(128), [], [fgbc])
        a = [T(ar.alloc(D, F32, name=f"ca{i}")) for i in range(8)]
        b = [T(ar.alloc(D, F32, name=f"cb{i}")) for i in range(8)]
        c = [T(ar.alloc(D, F32, name=f"cc{i}")) for i in range(8)]
        junk = T(ar.alloc(D, BF16, name="junkc"))
        ss = [T(ar.alloc(1, F32, name=f"ssc{i}")) for i in range(8)]
        for t in range(NTL):
            i = t % 8
            r0 = t * 128
            self.dma("sp", a[i].ap, self.h2_d[r0:r0 + 128, :], [], [a[i]])
            self.idma(lambda e, o=b[i].ap, ix=self.dsti.ap[:, t, 0:1]: e.indirect_dma_start(out=o, out_offset=None, in_=self.Y_d[:, :],
                      in_offset=bass.IndirectOffsetOnAxis(ap=ix, axis=0)), 1 << 19, [self.dsti], [b[i]])
            self.idma(lambda e, o=c[i].ap, ix=self.dsti.ap[:, t, 1:2]: e.indirect_dma_start(out=o, out_offset=None, in_=self.Y_d[:, :],
                      in_offset=bass.IndirectOffsetOnAxis(ap=ix, axis=0)), 1 << 19, [self.dsti], [c[i]])
            self.tt("dve", a[i].ap, a[i].ap, b[i].ap, ALU.add, [a[i], b[i]], [a[i]])
            self.tt("dve", a[i].ap, a[i].ap, c[i].ap, ALU.add, [a[i], c[i]], [a[i]])
            self.act(junk.ap, a[i].ap, AF.Square, [a[i]], [junk, ss[i]], accum_out=ss[i].ap)
            self.act(ss[i].ap, ss[i].ap, AF.Ln, [ss[i], self.eps_t], [ss[i]], scale=1.0 / D, bias=self.eps_t.ap)
            self.act(ss[i].ap, ss[i].ap, AF.Exp, [ss[i]], [ss[i]], scale=-0.5)
            self.stt(b[i].ap, a[i].ap, ss[i].ap[:, 0:1], fgbc.ap, ALU.mult, ALU.mult, [a[i], ss[i], fgbc], [b[i]])
            self.dma_out("sp", self.out_d[r0:r0 + 128, :], b[i].ap, [b[i]])


def _prep_inputs(inputs, core, nseq):
    m = {}
    x = np.ascontiguousarray(inputs["x"][core * nseq:(core + 1) * nseq]).reshape(nseq * SEQ, D)
    m["x"] = x
    m["meta_tokens"] = np.ascontiguousarray(inputs["meta_tokens"])
    for k in ["norm1_g", "w_in", "hg_norm_g", "conv_w", "conv_b", "lru_w_r", "lru_b_r", "lru_w_i", "lru_b_i",
              "lru_lambda", "w_up_a", "w_up_b", "w_out", "norm2_g", "w_group", "b_group", "w_router", "w_gate", "w_up", "w_down"]:
        m[k] = np.ascontiguousarray(inputs[k][0])
    m["b_router"] = np.ascontiguousarray(inputs["b_router"][0]).reshape(32)
    m["hg_lower_bounds"] = np.ascontiguousarray(inputs["hg_lower_bounds"])
    m["final_g"] = np.ascontiguousarray(inputs["final_g"])
    return m


def kernel(**inputs):
    nseq = inputs["x"].shape[0] // NCORES
    kb = KB(nseq)
    nc = kb.build()
    in_maps = [_prep_inputs(inputs, c, nseq) for c in range(NCORES)]
    res = run_bass_kernel_spmd(nc, in_maps, core_ids=list(range(NCORES)))
    out = np.concatenate([r["out"].reshape(nseq, SEQ, D) for r in res.results], axis=0)
    return out.astype(np.float32)
```

```python
from contextlib import ExitStack
import numpy as np
import concourse.bass as bass
import concourse.mybir as mybir
from concourse.bass_utils import run_bass_kernel_spmd

F32 = mybir.dt.float32
BF16 = mybir.dt.bfloat16
I32 = mybir.dt.int32
AF = mybir.ActivationFunctionType
ALU = mybir.AluOpType
AX = mybir.AxisListType

D = 1024
SEQ = 2048
NMETA = 16
DIN = 6144
NE = 32
DE = 512
EPS = 1e-6
NCORES = 8
BLK = 512

ENGS = ("sp", "act", "dve", "pool", "pe")
SEM_ROLL = 30000
DMA_RING = 8


ACT_SETS = {
    AF.Exp: (0, 6, 22), AF.Tanh: (0, 2, 8, 10, 11, 12, 18, 19, 20), AF.Sigmoid: (2, 21), AF.Sqrt: (3, 23),
    AF.Ln: (5, 6), AF.Gelu_apprx_tanh: (11,), AF.Silu: (18,),
}


class Buf:
    __slots__ = ("name", "lw", "rd", "excl")

    def __init__(self, name=""):
        self.name = name
        self.lw = None
        self.rd = []
        self.excl = False


class Tl:
    __slots__ = ("ap", "b")

    def __init__(self, ap, name=""):
        self.ap = ap
        self.b = Buf(name)


class Op:
    __slots__ = ("eng", "fn", "deps", "dma", "has_dep", "sem", "val", "prewait", "alld", "cost", "lat", "seg", "gi", "tbl", "preload")

    def __init__(self, eng, fn, dma):
        self.eng = eng
        self.fn = fn
        self.dma = dma
        self.deps = []
        self.alld = []
        self.has_dep = False
        self.sem = None
        self.val = 0
        self.prewait = None
        self.cost = 100.0
        self.lat = 0.0
        self.seg = 0
        self.gi = 0
        self.tbl = None
        self.preload = False


def _b(x):
    return x.b if isinstance(x, Tl) else x


class Sched:
    def __init__(self, nc):
        self.nc = nc
        self.ops = {e: [] for e in ENGS}
        self.allbufs = []
        self.seg = 0
        self.gi = 0
        self.reorder = True

    def op(self, eng, fn, reads=(), writes=(), dma=False, cost=None, lat=None):
        reads = [_b(x) for x in reads]
        writes = [_b(x) for x in writes]
        writes = writes + [b for b in reads if b.excl]
        reads = [b for b in reads if not b.excl]
        o = Op(eng, fn, dma)
        deps = {}
        for b in reads:
            if b.lw is not None:
                deps[id(b.lw)] = b.lw
        for b in writes:
            if b.lw is not None:
                deps[id(b.lw)] = b.lw
            for r in b.rd:
                deps[id(r)] = r
        out = []
        for d in deps.values():
            if (not d.dma) and d.eng == eng and not dma and eng == "pe":
                continue
            out.append(d)
        o.deps = out
        o.alld = list(deps.values())
        o.seg = self.seg
        o.gi = self.gi
        self.gi += 1
        if cost is not None:
            o.cost = cost
        if lat is not None:
            o.lat = lat
        for b in reads:
            b.rd.append(o)
        for b in writes:
            b.lw = o
            b.rd = []
        self.ops[eng].append(o)
        return o

    def list_schedule(self):
        import heapq
        allops = sorted((o for e in ENGS for o in self.ops[e]), key=lambda o: o.gi)
        segs = {}
        for o in allops:
            segs.setdefault(o.seg, []).append(o)
        new = {e: [] for e in ENGS}
        SEMLAT = 250.0
        cur_tbl = [-1]
        for sg in sorted(segs):
            ops = segs[sg]
            if len(ops) < 40:
                for o in ops:
                    new[o.eng].append(o)
                continue
            idx = {id(o): i for i, o in enumerate(ops)}
            n = len(ops)
            preds = [[idx[id(d)] for d in o.alld if id(d) in idx] for o in ops]
            succ = [[] for _ in range(n)]
            for i, ps in enumerate(preds):
                for p in ps:
                    succ[p].append(i)
            prio = [0.0] * n
            for i in range(n - 1, -1, -1):
                o = ops[i]
                m = 0.0
                for s_ in succ[i]:
                    if prio[s_] > m:
                        m = prio[s_]
                prio[i] = m + o.cost + o.lat
            indeg = [len(p) for p in preds]
            ready_t = [0.0] * n
            fin = [0.0] * n
            free = {e: 0.0 for e in ENGS}
            wait_h = {e: [] for e in ENGS}
            rdy_h = {e: [] for e in ENGS}
            for i in range(n):
                if indeg[i] == 0:
                    heapq.heappush(wait_h[ops[i].eng], (0.0, i))
            done = 0
            while done < n:
                best = None
                for e in ENGS:
                    t = free[e]
                    if rdy_h[e]:
                        cand = t
                    elif wait_h[e]:
                        cand = max(t, wait_h[e][0][0])
                    else:
                        continue
                    if best is None or cand < best[0]:
                        best = (cand, e)
                t, e = best
                while wait_h[e] and wait_h[e][0][0] <= t:
                    rt, i = heapq.heappop(wait_h[e])
                    heapq.heappush(rdy_h[e], (-prio[i], i))
                sw = 0.0
                if e == "act" and len(rdy_h[e]) > 1:
                    pick = None
                    for ent in rdy_h[e]:
                        tb = ops[ent[1]].tbl
                        if tb is None or cur_tbl[0] in tb:
                            if pick is None or ent < pick:
                                pick = ent
                    if pick is None:
                        pick = min(rdy_h[e])
                    rdy_h[e].remove(pick)
                    heapq.heapify(rdy_h[e])
                    i = pick[1]
                else:
                    _, i = heapq.heappop(rdy_h[e])
                if e == "act" and ops[i].tbl is not None and cur_tbl[0] not in ops[i].tbl and wait_h[e]:
                    alt = None
                    for ent in wait_h[e]:
                        if ent[0] <= t + 1300.0:
                            tb = ops[ent[1]].tbl
                            if tb is None or cur_tbl[0] in tb:
                                if alt is None or ent < alt:
                                    alt = ent
                    if alt is not None:
                        heapq.heappush(rdy_h[e], (-prio[i], i))
                        wait_h[e].remove(alt)
                        heapq.heapify(wait_h[e])
                        i = alt[1]
                o = ops[i]
                if e == "act" and o.tbl is not None and cur_tbl[0] not in o.tbl:
                    cur_tbl[0] = o.tbl[0]
                    sw = 1300.0
                start = max(t, ready_t[i]) + sw
                free[e] = start + o.cost
                fin[i] = start + o.cost + o.lat
                new[e].append(o)
                done += 1
                for s_ in succ[i]:
                    so = ops[s_]
                    r = fin[i] + (SEMLAT if (so.eng != e or o.dma) else 60.0)
                    if so.eng == e and e == "pe" and not o.dma:
                        r = start + o.cost
                    if r > ready_t[s_]:
                        ready_t[s_] = r
                    indeg[s_] -= 1
                    if indeg[s_] == 0:
                        heapq.heappush(wait_h[so.eng], (ready_t[s_], s_))
        self.ops = new

    def emit(self, stack):
        nc = self.nc
        if self.reorder:
            self.list_schedule()
        pos = {}
        for e in ENGS:
            for i, o in enumerate(self.ops[e]):
                pos[id(o)] = i
        for e in ENGS:
            for o in self.ops[e]:
                best = {}
                need = []
                for d in o.deps:
                    if d.dma:
                        need.append(d)
                    else:
                        c = best.get(d.eng)
                        if c is None or pos[id(d)] > pos[id(c)]:
                            best[d.eng] = d
                need.extend(best.values())
                o.deps = need
                for d in need:
                    d.has_dep = True
        esems = {}
        for e in ENGS:
            nmile = sum(1 for o in self.ops[e] if (not o.dma) and o.has_dep)
            ngen = nmile // SEM_ROLL + 1
            esems[e] = [stack.enter_context(nc.semaphore(f"s_{e}_{g}")) for g in range(ngen)]
        rsems = {}
        for e in ENGS:
            if any(o.dma for o in self.ops[e]):
                rsems[e] = [stack.enter_context(nc.semaphore(f"r_{e}_{j}")) for j in range(DMA_RING)]
        finals = []
        for e in ENGS:
            cnt = 0
            dcnt = 0
            for o in self.ops[e]:
                if o.dma:
                    j = dcnt % DMA_RING
                    o.sem = rsems[e][j]
                    o.val = 16 * (dcnt // DMA_RING + 1)
                    if dcnt >= DMA_RING:
                        o.prewait = (o.sem, o.val - 16)
                    dcnt += 1
                elif o.has_dep:
                    o.sem = esems[e][cnt // SEM_ROLL]
                    o.val = cnt % SEM_ROLL + 1
                    cnt += 1
            if e in rsems:
                for j in range(min(dcnt, DMA_RING)):
                    uses = (dcnt - j + DMA_RING - 1) // DMA_RING
                    finals.append((rsems[e][j], 16 * uses))
        block = stack.enter_context(nc.Block())

        def run(e, name):
            seen = {}
            for o in self.ops[name]:
                waits = []
                if o.prewait is not None:
                    waits.append(o.prewait)
                for d in o.deps:
                    waits.append((d.sem, d.val))
                for s, v in waits:
                    k = id(s)
                    if seen.get(k, 0) >= v:
                        continue
                    seen[k] = v
                    e.wait_ge(s, v)
                ins = o.fn(e)
                if o.dma:
                    ins.then_inc(o.sem, 16)
                elif o.has_dep:
                    ins.then_inc(o.sem, 1)
            if name == "sp":
                for s, v in finals:
                    e.wait_ge(s, v)

        @block.sync
        def _(e):
            run(e, "sp")

        @block.scalar
        def _(e):
            run(e, "act")

        @block.vector
        def _(e):
            run(e, "dve")

        @block.gpsimd
        def _(e):
            run(e, "pool")

        @block.tensor
        def _(e):
            run(e, "pe")


class Arena:
    def __init__(self, ap):
        self.ap = ap
        self.off = 0
        self.W = ap.shape[1]

    def alloc(self, free, dt=F32, parts=128, name=""):
        if isinstance(free, int):
            free = (free,)
        n = 1
        for f in free:
            n *= f
        two = dt == BF16
        words = (n + 1) // 2 if two else n
        assert self.off + words <= self.W, f"arena overflow {name} {self.off}+{words}>{self.W}"
        a = self.ap[0:parts, self.off:self.off + words]
        self.off += words
        if two:
            a = a.bitcast(BF16)
            if n != 2 * words:
                a = a[:, 0:n]
        elif dt != F32:
            a = a.bitcast(dt)
        if len(free) == 2:
            a = a.rearrange("p (a b) -> p a b", a=free[0])
        elif len(free) == 3:
            a = a.rearrange("p (a b c) -> p a b c", a=free[0], b=free[1])
        return Tl(a, name)


class KB:
    def __init__(self, nseq, dbg=(), stop=None):
        self.nseq = nseq
        self.ntok = nseq * SEQ
        self.dbg = set(dbg)
        self.stop = stop
        self.nc = bass.Bass("TRN2", target_bir_lowering=False)
        self.S = Sched(self.nc)

    @staticmethod
    def _n(ap):
        n = 1
        for d in ap.shape[1:]:
            n *= d
        return n

    def _ew_cost(self, eng, out, in_=None, mult=1.0):
        n = self._n(out)
        if eng == "act":
            return 190.0 + 0.72 * n * mult
        if eng == "dve":
            return 75.0 + 1.04 * n * mult
        return 130.0 + 1.2 * n * mult

    def act(self, out, in_, func, R, W, **kw):
        o = self.S.op("act", lambda e: e.activation(out=out, in_=in_, func=func, **kw), R, W, cost=self._ew_cost("act", out), lat=60.0)
        o.tbl = ACT_SETS.get(func)
        return o

    def ts(self, eng, out, in0, s1, s2, op0, op1, R, W):
        c = self._ew_cost(eng, out, mult=0.6 if eng == "dve" else 1.0)
        if s2 is None:
            return self.S.op(eng, lambda e: e.tensor_scalar(out=out, in0=in0, scalar1=s1, scalar2=None, op0=op0), R, W, cost=c, lat=60.0)
        return self.S.op(eng, lambda e: e.tensor_scalar(out=out, in0=in0, scalar1=s1, scalar2=s2, op0=op0, op1=op1), R, W, cost=c, lat=60.0)

    def tt(self, eng, out, in0, in1, op, R, W):
        return self.S.op(eng, lambda e: e.tensor_tensor(out=out, in0=in0, in1=in1, op=op), R, W, cost=self._ew_cost(eng, out), lat=60.0)

    def stt(self, out, in0, scalar, in1, op0, op1, R, W):
        return self.S.op("dve", lambda e: e.scalar_tensor_tensor(out=out, in0=in0, scalar=scalar, in1=in1, op0=op0, op1=op1), R, W, cost=self._ew_cost("dve", out), lat=60.0)

    def cp(self, eng, out, in_, R, W):
        c = self._ew_cost(eng, out, mult=0.7 if eng == "dve" else 1.0)
        if eng == "act":
            return self.S.op("act", lambda e: e.copy(out=out, in_=in_), R, W, cost=c, lat=60.0)
        return self.S.op(eng, lambda e: e.tensor_copy(out=out, in_=in_), R, W, cost=c, lat=60.0)

    def mm(self, out, lhsT, rhs, start, stop, R, W):
        n = self._n(out)
        c = (max(n, 64) * 0.43 + 12.0) * (4.0 if lhsT.dtype == F32 else 1.0)
        return self.S.op("pe", lambda e: e.matmul(out, lhsT=lhsT, rhs=rhs, start=start, stop=stop), R, W, cost=c, lat=220.0)

    def tr(self, out, in_, ident, R, W):
        c = 70.0 * (4.0 if in_.dtype == F32 else 1.0)
        return self.S.op("pe", lambda e: e.transpose(out, in_, ident), R, W, cost=c, lat=220.0)

    def scan(self, out, d0, d1, init, R, W):
        return self.S.op("dve", lambda e: e.tensor_tensor_scan(out=out, data0=d0, data1=d1, initial=init, op0=ALU.mult, op1=ALU.add), R, W,
                         cost=self._ew_cost("dve", out, mult=2.0), lat=60.0)

    def recip(self, out, in_, R, W):
        return self.S.op("dve", lambda e: e.reciprocal(out=out, in_=in_), R, W, cost=self._ew_cost("dve", out, mult=6.3), lat=60.0)

    def memset(self, eng, ap, val, W):
        return self.S.op(eng, lambda e: e.memset(ap, val), [], W, cost=self._ew_cost(eng, ap, mult=0.5))

    def dma(self, eng, out, in_, R, W, **kw):
        nbytes = self._n(out) * out.shape[0] * (2 if out.dtype == BF16 else 4)
        return self.S.op(eng, lambda e: e.dma_start(out=out, in_=in_, **kw), R, W, dma=True, cost=70.0, lat=2200.0 + nbytes / 120.0)

    def idma(self, fn, nbytes, R, W):
        return self.S.op("pool", fn, R, W, dma=True, cost=1100.0, lat=3000.0 + nbytes / 90.0)

    def dma_out(self, eng, out, in_, R, **kw):
        t = self.track(Tl(None, "dram_w"))
        return self.dma(eng, out, in_, R, [t], **kw)

    def barrier(self, tiles):
        S = self.S
        S.seg += 1
        allb = list(self._all_bufs)
        tok = self.bar_tok
        self.memset("dve", tok.ap[:, 0:4], 0.0, allb + [tok])
        self.memset("pool", tok.ap[:, 1:2], 0.0, [tok] + allb)
        self.S.op("act", lambda e: e.copy(out=tok.ap[:, 2:3], in_=tok.ap[:, 0:1]), [tok], [tok] + allb)
        self.S.op("pe", lambda e: e.matmul(self.pb[6].ap[0:1, 0:1], lhsT=self.bar_bf.ap[0:1, 0:1], rhs=self.bar_bf.ap[0:1, 0:1], start=True, stop=True), [tok, self.bar_bf], [self.pb[6]] + allb)
        self.dma("sp", self.bar_d[0:1, 0:4], tok.ap[0:1, 0:4], [tok] + allb, [tok])
        self.S.op("dve", lambda e: e.memset(tok.ap[:, 3:4], 0.0), [tok], [tok] + allb)
        S.seg += 1

    def track(self, t):
        self._all_bufs.append(t.b)
        return t

    def build(self):
        nc = self.nc
        S = self.S
        NT = self.ntok

        def din(name, shape, dt=F32):
            return nc.dram_tensor(name, list(shape), dt, kind="ExternalInput").ap()

        def dscr(name, shape, dt):
            kind = "ExternalOutput" if name in self.dbg else "Internal"
            return nc.dram_tensor(name, list(shape), dt, kind=kind).ap()

        I = {}
        I["x"] = din("x", [NT, D])
        I["meta_tokens"] = din("meta_tokens", [NMETA, D])
        I["norm1_g"] = din("norm1_g", [D])
        I["w_in"] = din("w_in", [D, DIN])
        I["hg_lower_bounds"] = din("hg_lower_bounds", [2, 512])
        I["hg_norm_g"] = din("hg_norm_g", [512])
        I["conv_w"] = din("conv_w", [4, D])
        I["conv_b"] = din("conv_b", [D])
        I["lru_w_r"] = din("lru_w_r", [8, 128, 128])
        I["lru_b_r"] = din("lru_b_r", [D])
        I["lru_w_i"] = din("lru_w_i", [8, 128, 128])
        I["lru_b_i"] = din("lru_b_i", [D])
        I["lru_lambda"] = din("lru_lambda", [D])
        I["w_up_a"] = din("w_up_a", [512, D])
        I["w_up_b"] = din("w_up_b", [D, D])
        I["w_out"] = din("w_out", [D, D])
        I["norm2_g"] = din("norm2_g", [D])
        I["w_group"] = din("w_group", [D, 4])
        I["b_group"] = din("b_group", [4])
        I["w_router"] = din("w_router", [4, D, 8])
        I["b_router"] = din("b_router", [32])
        I["w_gate"] = din("w_gate", [NE, D, DE])
        I["w_up"] = din("w_up", [NE, D, DE])
        I["w_down"] = din("w_down", [NE, DE, D])
        I["final_g"] = din("final_g", [D])
        self.I = I
        out_kind = "ExternalOutput"
        self.out_d = nc.dram_tensor("out", [NT, D], F32, kind=out_kind).ap()
        self.hnT_d = dscr("hnT_d", [D, NT], BF16)
        self.hnTm_d = dscr("hnTm_d", [D, NMETA], BF16)
        self.ta_d = dscr("ta_d", [D, NT], BF16)
        self.h2_d = dscr("h2_d", [NT, D], F32)
        self.hn2_d = dscr("hn2_d", [NT, D], BF16)
        self.bar_d = dscr("bar_d", [1, 4], F32)
        NBk = (2 * NT) // BLK + NE
        self.slot_d = dscr("slot_d", [NBk * BLK, 4], I32)
        self.Y_d = dscr("Y_d", [NBk * BLK, D], F32)
        self.DB = {}
        for nm in self.dbg:
            pass

        with ExitStack() as st:
            big = st.enter_context(nc.sbuf_tensor("arena", [128, 52000], F32))
            self.big = big
            self.pb = []
            for i in range(8):
                p = st.enter_context(nc.psum_tensor(f"pb{i}", [128, 512], F32))
                self.pb.append(Tl(p[:], f"pb{i}"))
                self.pb[-1].b.excl = True
            self._all_bufs = [p.b for p in self.pb]
            self.ar = Arena(big[:])
            ar = self.ar
            self.bar_tok = ar.alloc(4, F32, name="bar_tok")
            self.bar_bf = self.track(ar.alloc(2, BF16, name="bar_bf"))
            self.memset("dve", self.bar_bf.ap, 0.0, [self.bar_bf])
            self.consts()
            NTL = self.ntok // 128
            self.Mall = self.track(ar.alloc((NTL, 64), BF16, name="Mall"))
            self.wts = self.track(ar.alloc((NTL, 2), F32, name="wts"))
            self.NB = (2 * self.ntok) // BLK + NE
            self.bei = self.track(ar.alloc((self.NB, 2), I32, name="bei"))
            self.dsti = self.track(ar.alloc((NTL, 2), I32, name="dsti"))
            base = ar.off
            self.phase_a1()
            if self.stop and self.stop.startswith("a1"):
                S.emit(st)
                return nc
            self.barrier(None)
            ar.off = base
            self.phase_a2()
            if self.stop and self.stop.startswith("a2"):
                S.emit(st)
                return nc
            self.barrier(None)
            ar.off = base
            self.phase_b0()
            if self.stop == "b0":
                S.emit(st)
                return nc
            self.barrier(None)
            ar.off = base
            self.phase_b()
            if self.stop == "b":
                S.emit(st)
                return nc
            self.barrier(None)
            ar.off = base
            self.phase_c()
            S.emit(st)
        return nc

    def consts(self):
        ar = self.ar
        T = self.track
        self.ident_bf = T(ar.alloc(128, BF16, name="ident_bf"))
        self.ident_f = T(ar.alloc(128, F32, name="ident_f"))
        self.ones_f = T(ar.alloc(128, F32, name="ones_f"))
        self.eps_t = T(ar.alloc(1, F32, name="eps"))
        self.one_t = T(ar.alloc(1, F32, name="one"))
        self.memset("pool", self.ident_f.ap, 1.0, [self.ident_f])
        self.S.op("pool", lambda e: e.affine_select(out=self.ident_f.ap, in_=self.ident_f.ap, pattern=[[1, 128]], compare_op=ALU.is_equal, fill=0.0, base=0, channel_multiplier=-1), [self.ident_f], [self.ident_f])
        self.cp("dve", self.ident_bf.ap, self.ident_f.ap, [self.ident_f], [self.ident_bf])
        self.memset("dve", self.ones_f.ap, 1.0, [self.ones_f])
        self.memset("dve", self.eps_t.ap, EPS, [self.eps_t])
        self.memset("dve", self.one_t.ap, 1.0, [self.one_t])

    def load_featT(self, dst, src1d, n):
        self.dma("sp", dst.ap, src1d.rearrange("(k p) -> p k", p=128), [], [dst], allow_slow_non_contiguous=True)

    def load_w_cast(self, W, dst_c0, src, src_c0, ncols):
        srcv = src.rearrange("(k p) c -> p k c", p=128)
        for c0 in range(0, ncols, 2048):
            n = min(2048, ncols - c0)
            self.dma("pool", W.ap[:, :, dst_c0 + c0:dst_c0 + c0 + n], srcv[:, :, src_c0 + c0:src_c0 + c0 + n], [], [W])

    def load_w_cols(self, W, dst_c0, src, src_c0, ncols, kcn, scaleT, stage, cscale=None):
        srcv = src.rearrange("(k p) c -> p k c", p=128)
        CB = 256
        for i, c0 in enumerate(range(0, ncols, CB)):
            stg = stage[self._stg % 2]
            self._stg += 1
            self.dma("sp", stg.ap[:, 0:kcn, :], srcv[:, :, src_c0 + c0:src_c0 + c0 + CB], [], [stg])
            for kc in range(kcn):
                eng = ("dve", "pool", "act")[kc % 3]
                o = W.ap[:, kc, dst_c0 + c0:dst_c0 + c0 + CB]
                if scaleT is None and cscale is not None:
                    self.ts("dve" if eng == "act" else eng, o, stg.ap[:, kc, :], float(cscale), None, ALU.mult, None, [stg], [W])
                elif scaleT is None:
                    self.cp(eng, o, stg.ap[:, kc, :], [stg], [W])
                elif eng == "act":
                    self.act(o, stg.ap[:, kc, :], AF.Copy, [stg, scaleT], [W], scale=scaleT.ap[:, kc:kc + 1])
                else:
                    self.ts(eng, o, stg.ap[:, kc, :], scaleT.ap[:, kc:kc + 1], None, ALU.mult, None, [stg, scaleT], [W])

    def phase_a1(self):
        ar = self.ar
        T = self.track
        I = self.I
        pb = self.pb
        self._stg = 0
        TT = 512
        W1 = T(ar.alloc((8, 3072), BF16, name="W1"))
        Wua = T(ar.alloc((4, 1024), BF16, name="Wua"))
        stage = [T(ar.alloc((8, 256), F32, name=f"stg{i}")) for i in range(2)]
        g1T = T(ar.alloc(8, F32, name="g1T"))
        self.load_featT(g1T, I["norm1_g"], 8)
        self.load_w_cast(W1, 0, I["w_in"], 0, 2048)
        self.load_w_cast(W1, 2048, I["w_in"], 4096, 1024)
        self.load_w_cast(Wua, 0, I["w_up_a"], 0, 1024)
        hlb0 = T(ar.alloc(4, F32)); hlb1 = T(ar.alloc(4, F32))
        lb = T(ar.alloc(4, F32, name="lb")); oml = T(ar.alloc(4, F32, name="oml")); noml = T(ar.alloc(4, F32, name="noml"))
        gnT = T(ar.alloc(4, F32, name="gnT"))
        self.load_featT(hlb0, I["hg_lower_bounds"][0, :], 4)
        self.load_featT(hlb1, I["hg_lower_bounds"][1, :], 4)
        self.load_featT(gnT, I["hg_norm_g"], 4)
        self.tt("dve", hlb0.ap, hlb0.ap, hlb1.ap, ALU.subtract, [hlb0, hlb1], [hlb0])
        self.act(lb.ap, hlb0.ap, AF.Sigmoid, [hlb0], [lb])
        self.ts("dve", oml.ap, lb.ap, -1.0, 1.0, ALU.mult, ALU.add, [lb], [oml])
        self.ts("dve", noml.ap, oml.ap, -1.0, None, ALU.mult, None, [oml], [noml])
        mask = T(ar.alloc(TT, F32, parts=64, name="mask"))
        self.memset("pool", mask.ap, 1.0, [mask])
        self.S.op("pool", lambda e: e.affine_select(out=mask.ap, in_=mask.ap, pattern=[[0, TT // 64], [1, 64]], compare_op=ALU.is_ge, fill=0.0, base=0, channel_multiplier=-1), [mask], [mask])
        rmask = T(ar.alloc(TT, F32, name="rmask"))
        self.memset("pool", rmask.ap, 1.0, [rmask])
        self.memset("pool", rmask.ap[:, 0::64], 0.0, [rmask])
        xt = [T(ar.alloc(D, F32, name=f"xt{i}")) for i in range(2)]
        xn = [T(ar.alloc(D, BF16, name=f"xn{i}")) for i in range(2)]
        junk = T(ar.alloc(D, BF16, name="junk"))
        ss = [T(ar.alloc(1, F32, name=f"ss{i}")) for i in range(2)]
        hnT_r = [T(ar.alloc((8, TT), BF16, name=f"hnT{i}")) for i in range(2)]
        self._hn = 0
        vt = T(ar.alloc((8, 512), BF16, parts=64, name="vt"))
        HB = []
        for h in range(4):
            d = dict(
                A=T(ar.alloc(TT, F32, name=f"A{h}")), B=T(ar.alloc(TT, F32, name=f"B{h}")), C=T(ar.alloc(TT, F32, name=f"C{h}")),
                qt=T(ar.alloc(TT, BF16, name=f"qt{h}")), kt=T(ar.alloc(TT, BF16, name=f"kt{h}")), kh=T(ar.alloc(TT, BF16, name=f"kh{h}")),
                sm=T(ar.alloc(TT, BF16, parts=64, name=f"sm{h}")), khT=T(ar.alloc((8, 128), BF16, parts=64, name=f"khT{h}")),
                eg=T(ar.alloc(8, F32, name=f"eg{h}")), S=T(ar.alloc(128, F32, name=f"S{h}")),
                Sb=[T(ar.alloc(128, BF16, name=f"Sb{h}{i}")) for i in range(2)], Sm=T(ar.alloc(128, F32, name=f"Sm{h}")),
                oa=T(ar.alloc(TT, BF16, name=f"oa{h}")),
            )
            d["sbi"] = 0
            HB.append(d)
        sga = [T(ar.alloc(TT, BF16, name=f"sga{i}")) for i in range(2)]
        tab = [T(ar.alloc(TT, BF16, name=f"tab{i}")) for i in range(2)]
        print("A1 arena used", ar.off)
        proj = [pb[0], pb[1], pb[7], pb[2], pb[3], pb[4], pb[5], pb[6]]
        self._pj = 0

        def nproj():
            p = proj[self._pj % 8]
            self._pj += 1
            return p
        self._po = 0
        self._ps = 0
        hnTm_v = self.hnTm_d.rearrange("(k p) t -> p k t", p=128)
        hnT_v = self.hnT_d.rearrange("(k p) t -> p k t", p=128)
        ta_v = self.ta_d.rearrange("(k p) t -> p k t", p=128)

        def tile(xrows, Tn, tok0, meta):
            nsub = max(1, Tn // 128)
            tsz = min(Tn, 128)
            CL = min(64, Tn)
            NCH = Tn // CL
            hnT = hnT_r[self._hn % 2]
            self._hn += 1
            for u in range(nsub):
                x_ = xt[u % 2]; xn_ = xn[u % 2]; ss_ = ss[u % 2]
                self.dma("sp", x_.ap[0:tsz, :], xrows[u * tsz:(u + 1) * tsz, :], [], [x_])
                self.act(junk.ap[0:tsz, :], x_.ap[0:tsz, :], AF.Square, [x_], [junk, ss_], accum_out=ss_.ap[0:tsz, :])
                self.act(ss_.ap[0:tsz, :], ss_.ap[0:tsz, :], AF.Ln, [ss_, self.eps_t], [ss_], scale=1.0 / D, bias=self.eps_t.ap[0:tsz, :])
                self.act(ss_.ap[0:tsz, :], ss_.ap[0:tsz, :], AF.Exp, [ss_], [ss_], scale=-0.5)
                self.ts("dve", xn_.ap[0:tsz, :], x_.ap[0:tsz, :], ss_.ap[0:tsz, 0:1], None, ALU.mult, None, [x_, ss_], [xn_])
                P_tr = nproj()
                ptr = P_tr.ap.bitcast(BF16).rearrange("p (k t) -> p k t", k=8)
                for kc in range(8):
                    self.tr(ptr[:, kc, 0:tsz], xn_.ap[0:tsz, kc * 128:(kc + 1) * 128], self.ident_bf.ap[0:tsz, 0:tsz], [xn_, self.ident_bf], [P_tr])
                self.tt("dve", hnT.ap[:, :, u * tsz:(u + 1) * tsz], ptr[:, :, 0:tsz], g1T.ap.unsqueeze(2).to_broadcast([128, 8, tsz]), ALU.mult, [P_tr, g1T], [hnT])
            if meta:
                self.dma_out("sp", hnTm_v, hnT.ap[:, :, 0:Tn], [hnT])
            else:
                self.dma_out("sp", hnT_v[:, :, tok0:tok0 + Tn], hnT.ap[:, :, 0:Tn], [hnT])
            for c in range(NCH):
                p = nproj()
                for kc in range(8):
                    self.mm(p.ap[0:CL, :], hnT.ap[:, kc, c * CL:(c + 1) * CL], W1.ap[:, kc, 1024:1536], kc == 0, kc == 7, [hnT, W1], [p])
                self.cp("act", vt.ap[0:CL, c, :], p.ap[0:CL, :], [p], [vt])
            for h in range(4):
                hb = HB[h]
                A, B, C = hb["A"], hb["B"], hb["C"]
                p = nproj()
                for kc in range(8):
                    self.mm(p.ap[:, 0:Tn], W1.ap[:, kc, 512 + h * 128:512 + (h + 1) * 128], hnT.ap[:, kc, 0:Tn], kc == 0, kc == 7, [hnT, W1], [p])
                self.act(A.ap[:, 0:Tn], p.ap[:, 0:Tn], AF.Sigmoid, [p], [A])
                self.act(B.ap[:, 0:Tn], A.ap[:, 0:Tn], AF.Ln, [A, oml, lb], [B], scale=oml.ap[:, h:h + 1], bias=lb.ap[:, h:h + 1])
                self.ts("dve", A.ap[:, 0:Tn], A.ap[:, 0:Tn], noml.ap[:, h:h + 1], oml.ap[:, h:h + 1], ALU.mult, ALU.add, [A, noml, oml], [A])
                if meta:
                    self.scan(C.ap[:, 0:Tn], self.ones_f.ap[:, 0:Tn], B.ap[:, 0:Tn], 0.0, [B, self.ones_f], [C])
                else:
                    self.scan(C.ap[:, 0:Tn], rmask.ap[:, 0:Tn], B.ap[:, 0:Tn], 0.0, [B, rmask], [C])
                self.act(B.ap[:, 0:Tn], C.ap[:, 0:Tn], AF.Exp, [C], [B])
                self.act(C.ap[:, 0:Tn], C.ap[:, 0:Tn], AF.Exp, [C], [C], scale=-1.0)
                self.cp("pool", hb["eg"].ap[:, 0:NCH], B.ap[:, CL - 1:Tn:CL], [B], [hb["eg"]])
                self.tt("dve", hb["kt"].ap[:, 0:Tn], A.ap[:, 0:Tn], C.ap[:, 0:Tn], ALU.mult, [A, C], [hb["kt"]])
                self.tt("dve", hb["kh"].ap[:, 0:Tn].rearrange("p (c t) -> p c t", c=NCH), hb["kt"].ap[:, 0:Tn].rearrange("p (c t) -> p c t", c=NCH),
                        hb["eg"].ap[:, 0:NCH].unsqueeze(2).to_broadcast([128, NCH, CL]), ALU.mult, [hb["kt"], hb["eg"]], [hb["kh"]])
                P_ktr = nproj()
                pk = P_ktr.ap.bitcast(BF16).rearrange("p (c d) -> p c d", c=8)
                for c in range(NCH):
                    self.tr(pk[0:CL, c, :], hb["kh"].ap[:, c * CL:(c + 1) * CL], self.ident_bf.ap, [hb["kh"], self.ident_bf], [P_ktr])
                self.cp("act", hb["khT"].ap[0:CL, 0:NCH, :], pk[0:CL, 0:NCH, :], [P_ktr], [hb["khT"]])
                if meta:
                    continue
                p = nproj()
                for kc in range(8):
                    self.mm(p.ap[:, 0:Tn], W1.ap[:, kc, h * 128:(h + 1) * 128], hnT.ap[:, kc, 0:Tn], kc == 0, kc == 7, [hnT, W1], [p])
                self.tt("dve", hb["qt"].ap, p.ap, B.ap, ALU.mult, [p, B], [hb["qt"]])
                p = nproj()
                for kc in range(8):
                    self.mm(p.ap[:, 0:Tn], W1.ap[:, kc, 1536 + h * 128:1536 + (h + 1) * 128], hnT.ap[:, kc, 0:Tn], kc == 0, kc == 7, [hnT, W1], [p])
                self.act(B.ap, p.ap, AF.Sigmoid, [p], [B])
                P_sc = nproj()
                for c in range(NCH):
                    self.mm(P_sc.ap[0:64, c * 64:(c + 1) * 64], hb["kt"].ap[:, c * 64:(c + 1) * 64], hb["qt"].ap[:, c * 64:(c + 1) * 64], True, True, [hb["kt"], hb["qt"]], [P_sc])
                self.tt("dve", hb["sm"].ap, P_sc.ap[0:64, :], mask.ap, ALU.mult, [P_sc, mask], [hb["sm"]])
            for c in range(NCH):
                for h in range(4):
                    hb = HB[h]
                    hs = slice(h * 128, (h + 1) * 128)
                    Sb = hb["Sb"][hb["sbi"] % 2]
                    if not meta:
                        pob = nproj()
                        po = pob.ap[:, 0:64]
                        self.mm(po, vt.ap[0:64, c, hs], hb["sm"].ap[0:64, c * 64:(c + 1) * 64], True, False, [vt, hb["sm"]], [pob])
                        self.mm(po, Sb.ap, hb["qt"].ap[:, c * 64:(c + 1) * 64], False, True, [Sb, hb["qt"]], [pob])
                        self.cp("act", hb["A"].ap[:, c * 64:(c + 1) * 64], po, [pob], [hb["A"]])
                    psb = nproj()
                    psl = psb.ap[:, 0:128]
                    self.mm(psl, hb["khT"].ap[0:CL, c, :], vt.ap[0:CL, c, hs], True, True, [hb["khT"], vt], [psb])
                    if meta:
                        self.cp("dve", hb["Sm"].ap, psl, [psb], [hb["Sm"]])
                    else:
                        self.stt(hb["S"].ap, hb["S"].ap, hb["eg"].ap[:, c:c + 1], psl, ALU.mult, ALU.add, [hb["S"], hb["eg"], psb], [hb["S"]])
                        hb["sbi"] += 1
                        Sb2 = hb["Sb"][hb["sbi"] % 2]
                        self.cp("pool", Sb2.ap, hb["S"].ap, [hb["S"]], [Sb2])
            if meta:
                return
            for h in range(4):
                hb = HB[h]
                A, B, C = hb["A"], hb["B"], hb["C"]
                self.act(C.ap, A.ap, AF.Square, [A], [C])
                p = nproj()
                self.mm(p.ap, self.ones_f.ap, C.ap, True, True, [self.ones_f, C], [p])
                self.act(C.ap, p.ap, AF.Ln, [p, self.eps_t], [C], scale=1.0 / 128, bias=self.eps_t.ap)
                self.act(C.ap, C.ap, AF.Exp, [C], [C], scale=-0.5)
                self.stt(A.ap, A.ap, gnT.ap[:, h:h + 1], C.ap, ALU.mult, ALU.mult, [A, gnT, C], [A])
                self.tt("dve", hb["oa"].ap, A.ap, B.ap, ALU.mult, [A, B], [hb["oa"]])
            for mc in range(8):
                p = nproj()
                for kc in range(8):
                    self.mm(p.ap, W1.ap[:, kc, 2048 + mc * 128:2048 + (mc + 1) * 128], hnT.ap[:, kc, :], kc == 0, kc == 7, [hnT, W1], [p])
                sg = sga[mc % 2]
                self.act(sg.ap, p.ap, AF.Sigmoid, [p], [sg])
                p2 = nproj()
                for h in range(4):
                    self.mm(p2.ap, Wua.ap[:, h, mc * 128:(mc + 1) * 128], HB[h]["oa"].ap, h == 0, h == 3, [Wua, HB[h]["oa"]], [p2])
                tb_ = tab[mc % 2]
                self.tt("dve", tb_.ap, p2.ap, sg.ap, ALU.mult, [p2, sg], [tb_])
                self.dma_out("sp", ta_v[:, mc, tok0:tok0 + Tn], tb_.ap, [tb_])

        if self.stop == "a1_setup":
            return
        tile(I["meta_tokens"], NMETA, None, True)
        if self.stop == "a1_meta":
            return
        for s in range(self.nseq):
            for h in range(4):
                hb = HB[h]
                self.cp("dve", hb["S"].ap, hb["Sm"].ap, [hb["Sm"]], [hb["S"]])
                hb["sbi"] += 1
                self.cp("act", hb["Sb"][hb["sbi"] % 2].ap, hb["Sm"].ap, [hb["Sm"]], [hb["Sb"][hb["sbi"] % 2]])
            for j in range(SEQ // TT):
                tok0 = s * SEQ + j * TT
                tile(I["x"][tok0:tok0 + TT, :], TT, tok0, False)

    def phase_a2(self):
        ar = self.ar
        T = self.track
        I = self.I
        pb = self.pb
        TT = 512
        NTL = self.ntok // 128
        W2 = T(ar.alloc((8, 3072), BF16, name="W2"))
        Wub = T(ar.alloc((8, 1024), BF16, name="Wub"))
        Wout = T(ar.alloc((8, 1024), BF16, name="Wout"))
        wr = T(ar.alloc((8, 128), BF16, name="wr")); wi = T(ar.alloc((8, 128), BF16, name="wi"))
        stage = [T(ar.alloc((8, 256), F32, name=f"stg{i}")) for i in range(2)]
        g1T = T(ar.alloc(8, F32, name="g1T")); g2T = T(ar.alloc(8, F32, name="g2T"))
        self.load_featT(g1T, I["norm1_g"], 8)
        self.load_featT(g2T, I["norm2_g"], 8)
        self.load_w_cast(W2, 0, I["w_in"], 2048, 2048)
        self.load_w_cast(W2, 2048, I["w_in"], 5120, 1024)
        self.load_w_cast(Wub, 0, I["w_up_b"], 0, 1024)
        self.load_w_cast(Wout, 0, I["w_out"], 0, 1024)
        for (w_, src) in ((wr, I["lru_w_r"]), (wi, I["lru_w_i"])):
            self.dma("pool", w_.ap, src.rearrange("n i j -> i n j"), [], [w_])
        cwT = T(ar.alloc((4, 8), F32, name="cwT"))
        for j in range(4):
            self.dma("sp", cwT.ap[:, j, :], I["conv_w"][j, :].rearrange("(k p) -> p k", p=128), [], [cwT], allow_slow_non_contiguous=True)
        cbT = T(ar.alloc(8, F32)); brT = T(ar.alloc(8, F32)); biT = T(ar.alloc(8, F32)); lamT = T(ar.alloc(8, F32))
        cl = T(ar.alloc(8, F32)); cl2 = T(ar.alloc(8, F32))
        self.load_featT(cbT, I["conv_b"], 8)
        self.load_featT(brT, I["lru_b_r"], 8)
        self.load_featT(biT, I["lru_b_i"], 8)
        self.load_featT(lamT, I["lru_lambda"], 8)
        self.act(lamT.ap, lamT.ap, AF.Exp, [lamT], [lamT], scale=-1.0)
        self.act(lamT.ap, lamT.ap, AF.Ln, [lamT, self.one_t], [lamT], bias=self.one_t.ap)
        self.ts("dve", cl.ap, lamT.ap, -4.0, None, ALU.mult, None, [lamT], [cl])
        self.ts("dve", cl2.ap, lamT.ap, -8.0, None, ALU.mult, None, [lamT], [cl2])
        self.ts("dve", brT.ap, brT.ap, 0.5, None, ALU.mult, None, [brT], [brT])
        self.ts("dve", biT.ap, biT.ap, 0.5, None, ALU.mult, None, [biT], [biT])
        lnh = T(ar.alloc(1, F32, name="lnh"))
        self.memset("dve", lnh.ap, -0.6931471805599453, [lnh])
        g2bc = T(ar.alloc(D, F32, name="g2bc"))
        self.dma("sp", g2bc.ap, I["norm2_g"].partition_broadcast(128), [], [g2bc])
        Wr = T(ar.alloc((8, 36), F32, name="Wr"))
        self.dma("sp", Wr.ap[:, :, 0:4], I["w_group"].rearrange("(k p) c -> p k c", p=128), [], [Wr])
        for g in range(4):
            self.dma("sp", Wr.ap[:, :, 4 + g * 8:12 + g * 8], I["w_router"][g].rearrange("(k p) c -> p k c", p=128), [], [Wr])
        for kc in range(8):
            self.ts("dve", Wr.ap[:, kc, :], Wr.ap[:, kc, :], g2T.ap[:, kc:kc + 1], None, ALU.mult, None, [Wr, g2T], [Wr])
        brt = T(ar.alloc(36, F32, name="brt"))
        self.dma("sp", brt.ap[:, 0:4], I["b_group"].partition_broadcast(128), [], [brt])
        self.dma("sp", brt.ap[:, 4:36], I["b_router"].partition_broadcast(128), [], [brt])
        hist = [T(ar.alloc(3, F32, name=f"hist{c}")) for c in range(8)]
        histm = [T(ar.alloc(3, F32, name=f"histm{c}")) for c in range(8)]
        hst = [T(ar.alloc(1, F32, name=f"hst{c}")) for c in range(8)]
        hstm = [T(ar.alloc(1, F32, name=f"hstm{c}")) for c in range(8)]
        for c in range(8):
            self.memset("dve", hist[c].ap, 0.0, [hist[c]])
            self.memset("dve", hst[c].ap, 0.0, [hst[c]])
        hnT = T(ar.alloc((8, TT), BF16, name="hnT"))
        lxb = [T(ar.alloc(TT + 3, F32, name=f"lxb{i}")) for i in range(2)]
        NR = 2
        xc = [T(ar.alloc(TT, F32, name=f"xc{i}")) for i in range(NR)]
        xcb = [T(ar.alloc(TT, BF16, name=f"xcb{i}")) for i in range(NR)]
        Rb = [T(ar.alloc(TT, F32, name=f"R{i}")) for i in range(NR)]
        Ib = [T(ar.alloc(TT, F32, name=f"I{i}")) for i in range(NR)]
        Ab = [T(ar.alloc(TT, F32, name=f"Ab{i}")) for i in range(NR)]
        Hb = [T(ar.alloc(TT, F32, name=f"H{i}")) for i in range(NR)]
        ob = [T(ar.alloc(TT, BF16, name=f"ob{c}")) for c in range(8)]
        tat = T(ar.alloc((8, TT), BF16, name="tat"))
        sgb = [T(ar.alloc(TT, BF16, name=f"sgb{i}")) for i in range(2)]
        t2 = [T(ar.alloc(TT, F32, name=f"t2{i}")) for i in range(2)]
        mg = [T(ar.alloc(TT, BF16, name=f"mg{c}")) for c in range(8)]
        xr = [T(ar.alloc(D, F32, name=f"xr{i}")) for i in range(2)]
        hn2t = [T(ar.alloc(D, BF16, name=f"hn2t{i}")) for i in range(2)]
        h2T = T(ar.alloc((8, 128), F32, name="h2T"))
        ss = [T(ar.alloc(1, F32, name=f"ss2{i}")) for i in range(2)]
        rt = [dict(lg=T(ar.alloc(36, F32)), gmax=T(ar.alloc(1, F32)), gm=T(ar.alloc(4, F32)), ge=T(ar.alloc(4, F32)), gsum=T(ar.alloc(1, F32)),
                   pen=T(ar.alloc(4, F32)), ml=T(ar.alloc(32, F32)), top=T(ar.alloc(8, F32)), d=T(ar.alloc(1, F32))) for i in range(2)]
        print("A2 arena used", ar.off)
        proj = [pb[0], pb[1], pb[7], pb[5], pb[6], pb[2], pb[3], pb[4]]
        self._pj = 0

        def nproj():
            p = proj[self._pj % 8]
            self._pj += 1
            return p
        hnTm_v = self.hnTm_d.rearrange("(k p) t -> p k t", p=128)
        hnT_v = self.hnT_d.rearrange("(k p) t -> p k t", p=128)
        ta_v = self.ta_d.rearrange("(k p) t -> p k t", p=128)
        Mall, wts = self.Mall, self.wts
        BIG = 1.0e30

        def tile(Tn, tok0, meta):
            if meta:
                self.dma("sp", hnT.ap[:, :, 0:Tn], hnTm_v, [], [hnT])
            else:
                self.dma("sp", hnT.ap[:, :, 0:Tn], hnT_v[:, :, tok0:tok0 + Tn], [], [hnT])
                self.dma("sp", tat.ap, ta_v[:, :, tok0:tok0 + Tn], [], [tat])
            for c in range(8):
                i = c % NR
                lx_ = lxb[c % 2]; xc_ = xc[i]; xcb_ = xcb[i]; R_ = Rb[i]; I_ = Ib[i]; A_ = Ab[i]; H_ = Hb[i]
                p = nproj()
                for kc in range(8):
                    self.mm(p.ap[:, 0:Tn], W2.ap[:, kc, c * 128:(c + 1) * 128], hnT.ap[:, kc, 0:Tn], kc == 0, kc == 7, [hnT, W2], [p])
                self.cp("pool", lx_.ap[:, 0:3], hist[c].ap, [hist[c]], [lx_])
                self.cp("act", lx_.ap[:, 3:3 + Tn], p.ap[:, 0:Tn], [p], [lx_])
                self.ts("dve", xc_.ap[:, 0:Tn], lx_.ap[:, 0:Tn], cwT.ap[:, 0, c:c + 1], cbT.ap[:, c:c + 1], ALU.mult, ALU.add, [lx_, cwT, cbT], [xc_])
                for j in range(1, 4):
                    self.stt(xc_.ap[:, 0:Tn], lx_.ap[:, j:j + Tn], cwT.ap[:, j, c:c + 1], xc_.ap[:, 0:Tn], ALU.mult, ALU.add, [lx_, cwT, xc_], [xc_])
                self.cp("pool", hist[c].ap, lx_.ap[:, Tn:Tn + 3], [lx_], [hist[c]])
                self.cp("pool", xcb_.ap[:, 0:Tn], xc_.ap[:, 0:Tn], [xc_], [xcb_])
                P_r = nproj(); P_i = nproj()
                self.mm(P_r.ap[:, 0:Tn], wr.ap[:, c, :], xcb_.ap[:, 0:Tn], True, True, [wr, xcb_], [P_r])
                self.mm(P_i.ap[:, 0:Tn], wi.ap[:, c, :], xcb_.ap[:, 0:Tn], True, True, [wi, xcb_], [P_i])
                self.act(R_.ap[:, 0:Tn], P_r.ap[:, 0:Tn], AF.Tanh, [P_r, brT], [R_], bias=brT.ap[:, c:c + 1], scale=0.5)
                self.act(I_.ap[:, 0:Tn], P_i.ap[:, 0:Tn], AF.Tanh, [P_i, biT], [I_], bias=biT.ap[:, c:c + 1], scale=0.5)
                self.act(A_.ap[:, 0:Tn], R_.ap[:, 0:Tn], AF.Exp, [R_, cl], [A_], scale=cl.ap[:, c:c + 1], bias=cl.ap[:, c:c + 1])
                self.act(R_.ap[:, 0:Tn], R_.ap[:, 0:Tn], AF.Exp, [R_, cl2], [R_], scale=cl2.ap[:, c:c + 1], bias=cl2.ap[:, c:c + 1])
                self.act(R_.ap[:, 0:Tn], R_.ap[:, 0:Tn], AF.Ln, [R_, self.one_t], [R_], scale=-1.0, bias=self.one_t.ap)
                self.act(R_.ap[:, 0:Tn], R_.ap[:, 0:Tn], AF.Exp, [R_, lnh], [R_], scale=0.5, bias=lnh.ap)
                self.stt(I_.ap[:, 0:Tn], I_.ap[:, 0:Tn], 1.0, xc_.ap[:, 0:Tn], ALU.add, ALU.mult, [I_, xc_], [I_])
                self.tt("dve", I_.ap[:, 0:Tn], I_.ap[:, 0:Tn], R_.ap[:, 0:Tn], ALU.mult, [I_, R_], [I_])
                self.scan(H_.ap[:, 0:Tn], A_.ap[:, 0:Tn], I_.ap[:, 0:Tn], hst[c].ap[:, 0:1], [A_, I_, hst[c]], [H_])
                self.cp("pool", hst[c].ap, H_.ap[:, Tn - 1:Tn], [H_], [hst[c]])
                if meta:
                    self.cp("pool", hstm[c].ap, hst[c].ap, [hst[c]], [hstm[c]])
                    self.cp("pool", histm[c].ap, hist[c].ap, [hist[c]], [histm[c]])
                    continue
                p = nproj()
                for kc in range(8):
                    self.mm(p.ap, W2.ap[:, kc, 1024 + c * 128:1024 + (c + 1) * 128], hnT.ap[:, kc, :], kc == 0, kc == 7, [hnT, W2], [p])
                self.act(R_.ap, p.ap, AF.Gelu_apprx_tanh, [p], [R_])
                self.stt(ob[c].ap, H_.ap, 0.5, R_.ap, ALU.mult, ALU.mult, [H_, R_], [ob[c]])
            if meta:
                return
            for mc in range(8):
                p = nproj()
                for kc in range(8):
                    self.mm(p.ap, W2.ap[:, kc, 2048 + mc * 128:2048 + (mc + 1) * 128], hnT.ap[:, kc, :], kc == 0, kc == 7, [hnT, W2], [p])
                sg = sgb[mc % 2]; t2_ = t2[mc % 2]
                self.act(sg.ap, p.ap, AF.Tanh, [p], [sg], scale=0.5)
                P_u = nproj()
                for c in range(8):
                    self.mm(P_u.ap, Wub.ap[:, c, mc * 128:(mc + 1) * 128], ob[c].ap, c == 0, c == 7, [Wub, ob[c]], [P_u])
                self.stt(t2_.ap, sg.ap, 1.0, P_u.ap, ALU.add, ALU.mult, [P_u, sg], [t2_])
                self.tt("pool", mg[mc].ap, t2_.ap, tat.ap[:, mc, :], ALU.add, [t2_, tat], [mg[mc]])
            for u in range(Tn // 128):
                t0 = tok0 + u * 128
                til = t0 // 128
                xr_ = xr[u % 2]; ss_ = ss[u % 2]; hn2_ = hn2t[u % 2]; r_ = rt[u % 2]
                self.dma("sp", xr_.ap, I["x"][t0:t0 + 128, :], [], [xr_])
                for hf in range(2):
                    p = nproj()
                    for mc in range(8):
                        self.mm(p.ap, mg[mc].ap[:, u * 128:(u + 1) * 128], Wout.ap[:, mc, hf * 512:(hf + 1) * 512], mc == 0, mc == 7, [mg[mc], Wout], [p])
                    self.tt("dve", xr_.ap[:, hf * 512:(hf + 1) * 512], xr_.ap[:, hf * 512:(hf + 1) * 512], p.ap, ALU.add, [xr_, p], [xr_])
                self.dma_out("sp", self.h2_d[t0:t0 + 128, :], xr_.ap, [xr_])
                self.act(hn2_.ap, xr_.ap, AF.Square, [xr_], [hn2_, ss_], accum_out=ss_.ap)
                self.act(ss_.ap, ss_.ap, AF.Ln, [ss_, self.eps_t], [ss_], scale=1.0 / D, bias=self.eps_t.ap)
                self.act(ss_.ap, ss_.ap, AF.Exp, [ss_], [ss_], scale=-0.5)
                self.stt(hn2_.ap, xr_.ap, ss_.ap[:, 0:1], g2bc.ap, ALU.mult, ALU.mult, [xr_, ss_, g2bc], [hn2_])
                self.dma_out("sp", self.hn2_d[t0:t0 + 128, :], hn2_.ap, [hn2_])
                P_t0 = nproj(); P_t1 = nproj()
                for kc in range(8):
                    P_t = P_t0 if kc < 4 else P_t1
                    self.tr(P_t.ap[:, (kc % 4) * 128:(kc % 4 + 1) * 128], xr_.ap[:, kc * 128:(kc + 1) * 128], self.ident_f.ap, [xr_, self.ident_f], [P_t])
                self.cp("act", h2T.ap[:, 0:4, :], P_t0.ap.rearrange("p (k t) -> p k t", k=4), [P_t0], [h2T])
                self.cp("dve", h2T.ap[:, 4:8, :], P_t1.ap.rearrange("p (k t) -> p k t", k=4), [P_t1], [h2T])
                p = nproj()
                for kc in range(8):
                    self.mm(p.ap[:, 0:36], h2T.ap[:, kc, :], Wr.ap[:, kc, :], kc == 0, kc == 7, [h2T, Wr], [p])
                lg = r_["lg"]
                self.stt(lg.ap, p.ap[:, 0:36], ss_.ap[:, 0:1], brt.ap, ALU.mult, ALU.add, [p, ss_, brt], [lg])
                self.S.op("dve", lambda e, o=r_["gmax"].ap, i=lg.ap[:, 0:4]: e.tensor_reduce(out=o, in_=i, axis=AX.X, op=ALU.max), [lg], [r_["gmax"]])
                self.ts("dve", r_["gm"].ap, lg.ap[:, 0:4], r_["gmax"].ap[:, 0:1], None, ALU.is_equal, None, [lg, r_["gmax"]], [r_["gm"]])
                self.ts("dve", r_["pen"].ap, r_["gm"].ap, BIG, -BIG, ALU.mult, ALU.add, [r_["gm"]], [r_["pen"]])
                self.ts("dve", r_["gmax"].ap, r_["gmax"].ap, -1.0, None, ALU.mult, None, [r_["gmax"]], [r_["gmax"]])
                self.act(r_["ge"].ap, lg.ap[:, 0:4], AF.Exp, [lg, r_["gmax"]], [r_["ge"], r_["gsum"]], bias=r_["gmax"].ap[:, 0:1], accum_out=r_["gsum"].ap)
                self.recip(r_["gsum"].ap, r_["gsum"].ap, [r_["gsum"]], [r_["gsum"]])
                self.tt("dve", r_["ml"].ap.rearrange("p (g e) -> p g e", g=4), lg.ap[:, 4:36].rearrange("p (g e) -> p g e", g=4),
                        r_["pen"].ap.unsqueeze(2).to_broadcast([128, 4, 8]), ALU.add, [lg, r_["pen"]], [r_["ml"]])
                self.S.op("dve", lambda e, o=r_["top"].ap, i=r_["ml"].ap: e.max(out=o, in_=i), [r_["ml"]], [r_["top"]])
                self.ts("dve", Mall.ap[:, til, 0:32], r_["ml"].ap, r_["top"].ap[:, 0:1], None, ALU.is_equal, None, [r_["ml"], r_["top"]], [Mall])
                self.ts("dve", Mall.ap[:, til, 32:64], r_["ml"].ap, r_["top"].ap[:, 1:2], None, ALU.is_equal, None, [r_["ml"], r_["top"]], [Mall])
                self.tt("dve", r_["d"].ap, r_["top"].ap[:, 1:2], r_["top"].ap[:, 0:1], ALU.subtract, [r_["top"]], [r_["d"]])
                self.act(r_["d"].ap, r_["d"].ap, AF.Exp, [r_["d"]], [r_["d"]])
                self.ts("dve", r_["d"].ap, r_["d"].ap, 1.0, None, ALU.add, None, [r_["d"]], [r_["d"]])
                self.recip(r_["d"].ap, r_["d"].ap, [r_["d"]], [r_["d"]])
                self.tt("dve", wts.ap[:, til, 0:1], r_["d"].ap, r_["gsum"].ap, ALU.mult, [r_["d"], r_["gsum"]], [wts])
                self.tt("dve", wts.ap[:, til, 1:2], r_["gsum"].ap, wts.ap[:, til, 0:1], ALU.subtract, [r_["gsum"], wts], [wts])

        tile(NMETA, None, True)
        for s_ in range(self.nseq):
            for c in range(8):
                self.cp("pool", hst[c].ap, hstm[c].ap, [hstm[c]], [hst[c]])
                self.cp("pool", hist[c].ap, histm[c].ap, [histm[c]], [hist[c]])
            for j in range(SEQ // TT):
                tile(TT, s_ * SEQ + j * TT, False)


    def phase_b0(self):
        ar = self.ar
        T = self.track
        pb = self.pb
        NTL = self.ntok // 128
        NB = self.NB
        NTk = self.ntok
        Mall, wts, bei = self.Mall, self.wts, self.bei
        W = NTL * 32
        Msum = T(ar.alloc((NTL, 32), BF16, name="Msum"))
        U = T(ar.alloc(128, BF16, name="U")); onesb = T(ar.alloc(128, BF16, name="onesb"))
        Uf = T(ar.alloc(128, F32, name="Uf"))
        cs = T(ar.alloc((NTL, 32), F32, name="cs")); rk = T(ar.alloc((NTL, 32), F32, name="rk"))
        pre = T(ar.alloc((NTL + 1, 32), F32, name="pre"))
        tmp = T(ar.alloc((NTL, 32), F32, name="tmp"))
        cnt = T(ar.alloc(32, F32)); cnti = T(ar.alloc(32, I32)); pcnt = T(ar.alloc(32, F32)); pend = T(ar.alloc(32, F32)); pst = T(ar.alloc(32, F32))
        dst = T(ar.alloc((NTL, 2), F32, name="dst")); dsti = self.dsti
        tokI = T(ar.alloc(NTL, I32, name="tokI"))
        row = T(ar.alloc((NTL, 2, 4), I32, name="row"))
        NSR = NB * BLK // 128
        sinit = T(ar.alloc((NSR, 4), I32, name="sinit"))
        bvi = T(ar.alloc(NB, I32)); bv = T(ar.alloc(NB, F32)); cmp_ = T(ar.alloc((NB, 32), F32, name="cmp")); bef = T(ar.alloc(NB, F32))
        print("B0 arena used", ar.off)
        self.tt("dve", Msum.ap, Mall.ap[:, :, 0:32], Mall.ap[:, :, 32:64], ALU.add, [Mall], [Msum])
        self.memset("pool", Uf.ap, 1.0, [Uf])
        self.S.op("pool", lambda e: e.affine_select(out=Uf.ap, in_=Uf.ap, pattern=[[1, 128]], compare_op=ALU.is_gt, fill=0.0, base=0, channel_multiplier=-1), [Uf], [Uf])
        self.cp("dve", U.ap, Uf.ap, [Uf], [U])
        self.memset("dve", onesb.ap, 1.0, [onesb])
        Mf = Msum.ap.rearrange("p t e -> p (t e)")
        csf = cs.ap.rearrange("p t e -> p (t e)")
        rkf = rk.ap.rearrange("p t e -> p (t e)")
        for c0 in range(0, W, 512):
            n = min(512, W - c0)
            self.mm(pb[0].ap[:, 0:n], onesb.ap, Mf[:, c0:c0 + n], True, True, [onesb, Msum], [pb[0]])
            self.cp("act", csf[:, c0:c0 + n], pb[0].ap[:, 0:n], [pb[0]], [cs])
            self.mm(pb[1].ap[:, 0:n], U.ap, Mf[:, c0:c0 + n], True, True, [U, Msum], [pb[1]])
            self.cp("dve", rkf[:, c0:c0 + n], pb[1].ap[:, 0:n], [pb[1]], [rk])
        self.memset("dve", pre.ap[:, 0, :], 0.0, [pre])
        for t in range(NTL):
            self.tt("dve", pre.ap[:, t + 1, :], pre.ap[:, t, :], cs.ap[:, t, :], ALU.add, [pre, cs], [pre])
        self.ts("dve", cnti.ap, pre.ap[:, NTL, :], float(BLK - 1), None, ALU.add, None, [pre], [cnti])
        self.ts("dve", cnti.ap, cnti.ap, 9, 9, ALU.arith_shift_right, ALU.logical_shift_left, [cnti], [cnti])
        self.cp("dve", pcnt.ap, cnti.ap, [cnti], [pcnt])
        self.scan(pend.ap, self.ones_f.ap[:, 0:32], pcnt.ap, 0.0, [pcnt, self.ones_f], [pend])
        self.tt("dve", pst.ap, pend.ap, pcnt.ap, ALU.subtract, [pend, pcnt], [pst])
        self.tt("dve", rk.ap, rk.ap, pre.ap[:, 0:NTL, :], ALU.add, [rk, pre], [rk])
        self.tt("dve", rk.ap, rk.ap, pst.ap.unsqueeze(1).to_broadcast([128, NTL, 32]), ALU.add, [rk, pst], [rk])
        for k in range(2):
            self.tt("dve", tmp.ap, Mall.ap[:, :, k * 32:(k + 1) * 32], rk.ap, ALU.mult, [Mall, rk], [tmp])
            self.S.op("dve", lambda e, o=dst.ap[:, :, k], i=tmp.ap: e.tensor_reduce(out=o, in_=i, axis=AX.X, op=ALU.add), [tmp], [dst])
        self.cp("dve", dsti.ap, dst.ap, [dst], [dsti])
        self.S.op("pool", lambda e: e.iota(tokI.ap, pattern=[[128, NTL]], base=0, channel_multiplier=1), [], [tokI])
        self.memset("dve", row.ap, 0, [row])
        for k in range(2):
            self.cp("dve", row.ap[:, :, k, 0], tokI.ap, [tokI], [row])
            self.ts("dve", row.ap[:, :, k, 1], tokI.ap, float(k * NTk), None, ALU.add, None, [tokI], [row])
            self.cp("dve", row.ap.bitcast(F32)[:, :, k, 2], wts.ap[:, :, k], [wts], [row])
        self.memset("pool", sinit.ap, 0, [sinit])
        self.memset("pool", sinit.ap[:, :, 1:2], 2 * NTk, [sinit])
        sl_init = T(Tl(None, "slot_init"))
        self.dma("sp", self.slot_d.rearrange("(p a) f -> p a f", p=128), sinit.ap, [sinit], [sl_init])
        for t in range(NTL):
            for k in range(2):
                tb = T(Tl(None, "slot_sc"))
                self.idma(lambda e, t=t, k=k: e.indirect_dma_start(out=self.slot_d[:, :], out_offset=bass.IndirectOffsetOnAxis(ap=dsti.ap[:, t, k:k + 1], axis=0),
                                                                  in_=row.ap[:, t, k, :], in_offset=None), 2048, [dsti, row, sl_init], [tb])
        self.S.op("pool", lambda e: e.iota(bvi.ap, pattern=[[BLK, NB]], base=0, channel_multiplier=0), [], [bvi])
        self.cp("dve", bv.ap, bvi.ap, [bvi], [bv])
        self.tt("dve", cmp_.ap, pend.ap.unsqueeze(1).to_broadcast([128, NB, 32]), bv.ap.unsqueeze(2).to_broadcast([128, NB, 32]), ALU.is_le, [pend, bv], [cmp_])
        self.S.op("dve", lambda e: e.tensor_reduce(out=bef.ap, in_=cmp_.ap, axis=AX.X, op=ALU.add), [cmp_], [bef])
        flg = T(ar.alloc(NB, F32, name="flg"))
        self.ts("dve", flg.ap, bef.ap, float(NE) - 0.5, 16384.0, ALU.is_gt, ALU.mult, [bef], [flg])
        self.ts("dve", bef.ap, bef.ap, float(NE - 1), None, ALU.min, None, [bef], [bef])
        pI = T(ar.alloc(NB, I32, name="pI")); pF = T(ar.alloc(NB, F32, name="pF"))
        self.S.op("pool", lambda e: e.iota(pI.ap, pattern=[[0, NB]], base=0, channel_multiplier=1), [], [pI])
        self.cp("dve", pF.ap, pI.ap, [pI], [pF])
        self.stt(bef.ap, bef.ap, 128.0, pF.ap, ALU.mult, ALU.add, [bef, pF], [bef])
        self.stt(bef.ap, bef.ap, 2.0, flg.ap, ALU.mult, ALU.add, [bef, flg], [bef])
        self.cp("dve", bei.ap[:, :, 0], bef.ap, [bef], [bei])
        self.ts("dve", bef.ap, bef.ap, 1.0, None, ALU.add, None, [bef], [bef])
        self.cp("dve", bei.ap[:, :, 1], bef.ap, [bef], [bei])

    def phase_b(self):
        ar = self.ar
        T = self.track
        I = self.I
        pb = self.pb
        NB = self.NB
        NTk = self.ntok
        bei = self.bei
        wg = [T(ar.alloc((8, 512), BF16, name=f"wg{i}")) for i in range(3)]
        wu = [T(ar.alloc((8, 512), BF16, name=f"wu{i}")) for i in range(3)]
        wd = [T(ar.alloc((4, 1024), BF16, name=f"wd{i}")) for i in range(3)]
        xg = [T(ar.alloc(D, BF16, name=f"xg{i}")) for i in range(8)]
        xT = [T(ar.alloc((8, 512), BF16, name=f"xT{i}")) for i in range(3)]
        sgt = [T(ar.alloc(512, F32, name=f"sgt{i}")) for i in range(2)]
        hT = [T(ar.alloc((4, 512), BF16, name=f"hT{i}")) for i in range(3)]
        ysb = [T(ar.alloc(D, F32, name=f"ysb{i}")) for i in range(4)]
        sl = [T(ar.alloc((4, 4), I32, name=f"sl{i}")) for i in range(3)]
        print("B arena used", ar.off)
        wgv = I["w_gate"].rearrange("e (p h k) f -> (e p h) (k f)", p=128, h=2)
        wuv = I["w_up"].rearrange("e (p h k) f -> (e p h) (k f)", p=128, h=2)
        wdv = I["w_down"].rearrange("e (p h k) f -> (e p h) (k f)", p=128, h=2)
        ring = [pb[0], pb[1], pb[7], pb[4], pb[5], pb[6], pb[2], pb[3]]
        cnt = dict(r=0, xg=0, y=0)

        def nbank():
            p = ring[cnt["r"] % 8]
            cnt["r"] += 1
            return p
        regs = {}

        def wload(b, dst, view):
            d2 = dst.ap.rearrange("p k f -> p (k f)")
            for half in range(2):
                def wl(e, o=d2[:, half * 2048:(half + 1) * 2048], src=view, ix=bei.ap[:, b, half:half + 1]):
                    if "wb" not in regs:
                        regs["wb"] = e.to_reg(NE * 256 - 1)
                    return e.indirect_dma_start(out=o, out_offset=None, in_=src, in_offset=bass.IndirectOffsetOnAxis(ap=ix, axis=0),
                                                bounds_check=regs["wb"], oob_is_err=False)
                self.idma(wl, 1 << 20, [bei], [dst])

        for b in range(NB):
            bb = b % 3
            sl_ = sl[bb]
            self.dma("sp", sl_.ap, self.slot_d[b * BLK:(b + 1) * BLK, :].rearrange("(s p) f -> p s f", p=128), [], [sl_])
            wload(b, wg[bb], wgv)
            wload(b, wu[bb], wuv)
            wload(b, wd[bb], wdv)
            xT_ = xT[bb]; hT_ = hT[bb]
            for s_ in range(4):
                xg_ = xg[cnt["xg"] % 8]; cnt["xg"] += 1
                self.idma(lambda e, o=xg_.ap, ix=sl_.ap[:, s_, 0:1]: e.indirect_dma_start(out=o, out_offset=None, in_=self.hn2_d[:, :],
                          in_offset=bass.IndirectOffsetOnAxis(ap=ix, axis=0)), 1 << 18, [sl_], [xg_])
                pt = nbank()
                ptv = pt.ap.bitcast(BF16).rearrange("p (k t) -> p k t", k=8)
                for kc in range(8):
                    self.tr(ptv[:, kc, :], xg_.ap[:, kc::8], self.ident_bf.ap, [xg_, self.ident_bf], [pt])
                self.cp("act" if s_ % 2 == 0 else "dve", xT_.ap[:, :, s_ * 128:(s_ + 1) * 128], ptv, [pt], [xT_])
            for fc in range(4):
                pg = nbank()
                pu = nbank()
                for kc in range(8):
                    self.mm(pg.ap, wg[bb].ap[:, kc, fc::4], xT_.ap[:, kc, :], kc == 0, kc == 7, [wg[bb], xT_], [pg])
                for kc in range(8):
                    self.mm(pu.ap, wu[bb].ap[:, kc, fc::4], xT_.ap[:, kc, :], kc == 0, kc == 7, [wu[bb], xT_], [pu])
                sg_ = sgt[fc % 2]
                self.act(sg_.ap, pg.ap, AF.Silu, [pg], [sg_])
                self.tt("dve", hT_.ap[:, fc, :], pu.ap, sg_.ap, ALU.mult, [pu, sg_], [hT_])
            for s_ in range(4):
                y_ = ysb[cnt["y"] % 4]; cnt["y"] += 1
                wsl = sl_.ap.bitcast(F32)[:, s_, 2:3]
                for hf in range(2):
                    p = nbank()
                    for fc in range(4):
                        self.mm(p.ap, hT_.ap[:, fc, s_ * 128:(s_ + 1) * 128], wd[bb].ap[:, fc, hf * 512:(hf + 1) * 512], fc == 0, fc == 3, [hT_, wd[bb]], [p])
                    if hf == 0:
                        self.act(y_.ap[:, 0:512], p.ap, AF.Copy, [p, sl_], [y_], scale=wsl)
                    else:
                        self.ts("dve", y_.ap[:, 512:1024], p.ap, wsl, None, ALU.mult, None, [p, sl_], [y_])
                r0 = b * BLK + s_ * 128
                self.dma_out("sp", self.Y_d[r0:r0 + 128, :], y_.ap, [y_])

    def phase_c(self):
        ar = self.ar
        T = self.track
        I = self.I
        NTL = self.ntok // 128
        NTk = self.ntok
        fgbc = T(ar.alloc(D, F32, name="fgbc"))
        self.dma("sp", fgbc.ap, I["final_g"].partition_broadcast(128), [], [fgbc])
        a = [T(ar.alloc(D, F32, name=f"ca{i}")) for i in range(4)]
        b = [T(ar.alloc(D, F32, name=f"cb{i}")) for i in range(4)]
        c = [T(ar.alloc(D, F32, name=f"cc{i}")) for i in range(4)]
        junk = T(ar.alloc(D, BF16, name="junkc"))
        ss = [T(ar.alloc(1, F32, name=f"ssc{i}")) for i in range(4)]
        for t in range(NTL):
            i = t % 4
            r0 = t * 128
            self.dma("sp", a[i].ap, self.h2_d[r0:r0 + 128, :], [], [a[i]])
            self.idma(lambda e, o=b[i].ap, ix=self.dsti.ap[:, t, 0:1]: e.indirect_dma_start(out=o, out_offset=None, in_=self.Y_d[:, :],
                      in_offset=bass.IndirectOffsetOnAxis(ap=ix, axis=0)), 1 << 19, [self.dsti], [b[i]])
            self.idma(lambda e, o=c[i].ap, ix=self.dsti.ap[:, t, 1:2]: e.indirect_dma_start(out=o, out_offset=None, in_=self.Y_d[:, :],
                      in_offset=bass.IndirectOffsetOnAxis(ap=ix, axis=0)), 1 << 19, [self.dsti], [c[i]])
            self.tt("dve", a[i].ap, a[i].ap, b[i].ap, ALU.add, [a[i], b[i]], [a[i]])
            self.tt("pool", a[i].ap, a[i].ap, c[i].ap, ALU.add, [a[i], c[i]], [a[i]])
            self.act(junk.ap, a[i].ap, AF.Square, [a[i]], [junk, ss[i]], accum_out=ss[i].ap)
            self.act(ss[i].ap, ss[i].ap, AF.Ln, [ss[i], self.eps_t], [ss[i]], scale=1.0 / D, bias=self.eps_t.ap)
            self.act(ss[i].ap, ss[i].ap, AF.Exp, [ss[i]], [ss[i]], scale=-0.5)
            self.stt(b[i].ap, a[i].ap, ss[i].ap[:, 0:1], fgbc.ap, ALU.mult, ALU.mult, [a[i], ss[i], fgbc], [b[i]])
            self.dma_out("sp", self.out_d[r0:r0 + 128, :], b[i].ap, [b[i]])


def _prep_inputs(inputs, core, nseq):
    m = {}
    x = np.ascontiguousarray(inputs["x"][core * nseq:(core + 1) * nseq]).reshape(nseq * SEQ, D)
    m["x"] = x
    m["meta_tokens"] = np.ascontiguousarray(inputs["meta_tokens"])
    for k in ["norm1_g", "w_in", "hg_norm_g", "conv_w", "conv_b", "lru_w_r", "lru_b_r", "lru_w_i", "lru_b_i",
              "lru_lambda", "w_up_a", "w_up_b", "w_out", "norm2_g", "w_group", "b_group", "w_router", "w_gate", "w_up", "w_down"]:
        m[k] = np.ascontiguousarray(inputs[k][0])
    m["b_router"] = np.ascontiguousarray(inputs["b_router"][0]).reshape(32)
    m["hg_lower_bounds"] = np.ascontiguousarray(inputs["hg_lower_bounds"])
    m["final_g"] = np.ascontiguousarray(inputs["final_g"])
    return m


def kernel(**inputs):
    nseq = inputs["x"].shape[0] // NCORES
    kb = KB(nseq)
    nc = kb.build()
    in_maps = [_prep_inputs(inputs, c, nseq) for c in range(NCORES)]
    res = run_bass_kernel_spmd(nc, in_maps, core_ids=list(range(NCORES)))
    out = np.concatenate([r["out"].reshape(nseq, SEQ, D) for r in res.results], axis=0)
    return out.astype(np.float32)
```
